# Optimizing a Trainium2 kernel written in Bass

```python
import math
import jax, jax.numpy as jnp
from jax import lax
import numpy as np

D_MODEL = 1024
BATCH = 4
SEQ = 8192
DEPTH = 1

HEAD_DIM = 64
HEADS_PER_GROUP = 4
DILATED_PATTERNS = ((128, 1), (512, 4), (2048, 16))
N_GROUPS_A = 3
N_HEADS_A = N_GROUPS_A * HEADS_PER_GROUP
ATTN_WIDTH = N_HEADS_A * HEAD_DIM
ATTN_OUT_WIDTH = HEADS_PER_GROUP * HEAD_DIM
ALIBI_SPAN = 8.0
MASK_VALUE = -1e30
CONV_WIDTH = 768
CONV_K = 3
N_BRANCHES = 2
IN_COLS = 3 * ATTN_WIDTH + 3 * CONV_WIDTH + N_BRANCHES * D_MODEL
N_EXPERT_GROUPS = 4
EXPERTS_PER_GROUP = 8
N_EXPERTS = N_EXPERT_GROUPS * EXPERTS_PER_GROUP
TOP_K_WITHIN = 2
EXPERT_FF = 512
RMS_EPS = 1e-6

kernel_name = "hybrid_dilated_attn_shortconv_hmoe_encoder"


def rmsnorm(x, g):
    xf = x.astype(jnp.float32)
    y = xf * lax.rsqrt(jnp.mean(xf * xf, axis=-1, keepdims=True) + RMS_EPS) * g.astype(jnp.float32)
    return y.astype(x.dtype)


def alibi_slopes(n):
    return np.array([2.0 ** (-ALIBI_SPAN * (i + 1) / n) for i in range(n)], dtype=np.float32)


def dilated_window_attention(q, k, v, window, dilation, slopes):
    B, S, H, E = q.shape
    half = window // (2 * dilation)
    blk = half
    L = S // dilation
    nblk = -(-L // blk)
    Lp = nblk * blk

    def strided(a):
        return a.reshape(B, L, dilation, H, E).transpose(0, 2, 1, 3, 4)

    qs = jnp.pad(strided(q), ((0, 0), (0, 0), (0, Lp - L), (0, 0), (0, 0)))
    qs = qs.reshape(B, dilation, nblk, blk, H, E)

    def neighbours(a):
        ap = jnp.pad(strided(a), ((0, 0), (0, 0), (blk, Lp - L + blk), (0, 0), (0, 0)))
        return jnp.concatenate(
            [ap[:, :, s * blk:s * blk + Lp].reshape(B, dilation, nblk, blk, H, E) for s in range(3)],
            axis=3)

    ks = neighbours(k)
    vs = neighbours(v)
    scores = jnp.einsum('bdnqhe,bdnkhe->bdnhqk', qs, ks).astype(jnp.float32) * (E ** -0.5)

    qi = jnp.arange(blk)[:, None]
    kc = jnp.arange(3 * blk)[None, :]
    delta = kc - blk - qi
    key_pos = jnp.arange(nblk)[:, None, None] * blk - blk + kc[None]
    valid = (jnp.abs(delta) <= half)[None] & (key_pos >= 0) & (key_pos < L)
    dist = (jnp.abs(delta) * dilation).astype(jnp.float32)
    scores = scores - slopes.astype(jnp.float32)[:, None, None] * dist
    scores = jnp.where(valid[:, None], scores, MASK_VALUE)

    m = jnp.max(scores, axis=-1, keepdims=True)
    p = jnp.exp(scores - m)
    denom = jnp.sum(p, axis=-1, keepdims=True)
    out = jnp.einsum('bdnhqk,bdnkhe->bdnqhe', p, vs.astype(jnp.float32))
    out = out / jnp.moveaxis(denom, 3, 4)
    lse = jnp.moveaxis((m + jnp.log(denom))[..., 0], 3, 4)

    out = out.reshape(B, dilation, Lp, H, E)[:, :, :L].transpose(0, 2, 1, 3, 4).reshape(B, S, H, E)
    lse = lse.reshape(B, dilation, Lp, H)[:, :, :L].transpose(0, 2, 1, 3).reshape(B, S, H)
    return out, lse


def dilated_attention_mixer(q, k, v):
    B, S, _ = q.shape
    shp = (B, S, N_GROUPS_A, HEADS_PER_GROUP, HEAD_DIM)
    q, k, v = q.reshape(shp), k.reshape(shp), v.reshape(shp)
    slopes = jnp.asarray(alibi_slopes(N_HEADS_A).reshape(N_GROUPS_A, HEADS_PER_GROUP))
    outs, lses = [], []
    for g, (window, dilation) in enumerate(DILATED_PATTERNS):
        o, l = dilated_window_attention(q[:, :, g], k[:, :, g], v[:, :, g], window, dilation, slopes[g])
        outs.append(o)
        lses.append(l)
    outs = jnp.stack(outs, axis=0)
    w = jax.nn.softmax(jnp.stack(lses, axis=0), axis=0)
    merged = jnp.sum(w[..., None] * outs, axis=0)
    return merged.reshape(B, S, ATTN_OUT_WIDTH).astype(q.dtype)


def short_conv_mixer(b_gate, c_gate, x_in, conv_w):
    u = c_gate * x_in
    kern = conv_w.astype(u.dtype)[:, None, :]
    conv = lax.conv_general_dilated(u, kern, window_strides=(1,), padding=((CONV_K // 2, CONV_K // 2),),
                                    dimension_numbers=('NWC', 'WIO', 'NWC'), feature_group_count=CONV_WIDTH)
    return b_gate * conv


def hierarchical_moe(h, w_route_group, b_route_group, w_route_expert, b_route_expert, w1, w3, w2):
    B, S, D = h.shape
    t = h.reshape(B * S, D)
    coarse = (t @ w_route_group).astype(jnp.float32) + b_route_group.astype(jnp.float32)
    fine = (t @ w_route_expert).astype(jnp.float32) + b_route_expert.astype(jnp.float32)
    fine = fine.reshape(-1, N_EXPERT_GROUPS, EXPERTS_PER_GROUP)
    p_group = jax.nn.softmax(coarse, axis=-1)
    g_idx = lax.top_k(coarse, 1)[1][:, 0]
    pg = jnp.take_along_axis(p_group, g_idx[:, None], axis=1)[:, 0]
    fine_sel = jnp.take_along_axis(fine, g_idx[:, None, None], axis=1)[:, 0]
    p_exp = jax.nn.softmax(fine_sel, axis=-1)
    top_vals, top_idx = lax.top_k(p_exp, TOP_K_WITHIN)
    gate_w = pg[:, None] * top_vals / jnp.sum(top_vals, axis=-1, keepdims=True)
    expert_id = g_idx[:, None] * EXPERTS_PER_GROUP + top_idx
    combine = jnp.sum(jax.nn.one_hot(expert_id, N_EXPERTS, dtype=jnp.float32) * gate_w[..., None], axis=1)
    combine = combine.astype(t.dtype)
    out = jnp.zeros_like(t)
    for e in range(N_EXPERTS):
        hid = jax.nn.silu(t @ w1[e]) * (t @ w3[e])
        out = out + combine[:, e:e + 1] * (hid @ w2[e])
    return out.reshape(B, S, D)


def setup_inputs(seed: int = 0) -> dict:
    key = jax.random.key(seed)
    ks = jax.random.split(key, 20)
    f32 = jnp.float32
    D = D_MODEL

    def nrm(k, shape, fan_in):
        return jax.random.normal(k, shape, f32) * (fan_in ** -0.5)

    return {
        "x": jax.random.normal(ks[0], (BATCH, SEQ, D), f32),
        "norm_mix_g": 1.0 + 0.02 * jax.random.normal(ks[1], (DEPTH, D), f32),
        "w_in": nrm(ks[2], (DEPTH, D, IN_COLS), D),
        "b_gate": 0.01 * jax.random.normal(ks[3], (DEPTH, N_BRANCHES * D), f32),
        "conv_w": nrm(ks[4], (DEPTH, CONV_K, CONV_WIDTH), CONV_K),
        "w_attn_out": nrm(ks[5], (DEPTH, ATTN_OUT_WIDTH, D), ATTN_OUT_WIDTH),
        "w_conv_out": nrm(ks[6], (DEPTH, CONV_WIDTH, D), CONV_WIDTH),
        "w_out": nrm(ks[7], (DEPTH, D, D), D),
        "norm_ffn_g": 1.0 + 0.02 * jax.random.normal(ks[8], (DEPTH, D), f32),
        "w_route_group": nrm(ks[9], (DEPTH, D, N_EXPERT_GROUPS), D),
        "b_route_group": 0.01 * jax.random.normal(ks[10], (DEPTH, N_EXPERT_GROUPS), f32),
        "w_route_expert": nrm(ks[11], (DEPTH, D, N_EXPERTS), D),
        "b_route_expert": 0.01 * jax.random.normal(ks[12], (DEPTH, N_EXPERTS), f32),
        "w1": nrm(ks[13], (DEPTH, N_EXPERTS, D, EXPERT_FF), D),
        "w3": nrm(ks[14], (DEPTH, N_EXPERTS, D, EXPERT_FF), D),
        "w2": nrm(ks[15], (DEPTH, N_EXPERTS, EXPERT_FF, D), EXPERT_FF),
        "norm_final_g": 1.0 + 0.02 * jax.random.normal(ks[16], (D,), f32),
    }


def reference(x, norm_mix_g, w_in, b_gate, conv_w, w_attn_out, w_conv_out, w_out, norm_ffn_g,
              w_route_group, b_route_group, w_route_expert, b_route_expert, w1, w3, w2, norm_final_g):
    B, S, D = x.shape
    split_at = [ATTN_WIDTH, 2 * ATTN_WIDTH, 3 * ATTN_WIDTH,
                3 * ATTN_WIDTH + CONV_WIDTH, 3 * ATTN_WIDTH + 2 * CONV_WIDTH, 3 * ATTN_WIDTH + 3 * CONV_WIDTH]
    for l in range(DEPTH):
        h = rmsnorm(x, norm_mix_g[l])
        proj = h @ w_in[l]
        q, k, v, bg, cg, xin, gate_logits = jnp.split(proj, split_at, axis=-1)
        y_a = dilated_attention_mixer(q, k, v) @ w_attn_out[l]
        y_b = short_conv_mixer(bg, cg, xin, conv_w[l]) @ w_conv_out[l]
        gates = jax.nn.sigmoid(gate_logits + b_gate[l]).reshape(B, S, N_BRANCHES, D)
        merged = gates[:, :, 0] * y_a + gates[:, :, 1] * y_b
        x = x + merged @ w_out[l]
        h2 = rmsnorm(x, norm_ffn_g[l])
        x = x + hierarchical_moe(h2, w_route_group[l], b_route_group[l], w_route_expert[l],
                                 b_route_expert[l], w1[l], w3[l], w2[l])
    return rmsnorm(x, norm_final_g)
```

```python
import numpy as np
from contextlib import ExitStack
import concourse.bass as bass
import concourse.mybir as mybir
from concourse.bass_utils import run_bass_kernel_spmd

F32 = mybir.dt.float32
BF16 = mybir.dt.bfloat16
AF = mybir.ActivationFunctionType
ALU = mybir.AluOpType
AX = mybir.AxisListType

D = 1024
U = 2048
HALO = 1024
EXT = U + 2 * HALO
NT_EXT = EXT // 128
NT_OWN = U // 128
DIL = (1, 4, 16)
NVT = (17, 20, 32)
VT_OFF = (0, 17, 37)
NVT_ALL = 69
NE = 32
FF = 512
IN_COLS = 6656
N_CORES = 8
SEQ = 8192
MASKV = -30000.0
STOP_PHASE = -1
SUBSTOP = ''


class _Stop(Exception):
    pass


def sl_(start, step, n=128):
    return slice(start, start + (n - 1) * step + 1, step)


def vt_local(g, r, kt):
    return r * (NVT[g] // DIL[g]) + kt


class _Rec:
    def __getattr__(self, name):
        def m(*a, **k):
            return (name, a, k)
        return m


REC = _Rec()


class Buf:
    __slots__ = ("name", "w", "r", "sem", "cnt")

    def __init__(self, name):
        self.name = name
        self.w = None
        self.r = {}
        self.sem = None
        self.cnt = 0


class Sched:
    CE = ("pe", "act", "dve", "pool")
    ALL = ("pe", "act", "dve", "pool", "sp")

    def __init__(self, nc, es):
        self.nc = nc
        self.es = es
        self.prog = {e: [] for e in self.ALL}
        self.esem = {e: es.enter_context(nc.semaphore("es_" + e)) for e in self.CE}
        self.ecnt = {e: 0 for e in self.CE}
        self.waited = {e: {} for e in self.ALL}
        self.nsem = 0
        self.ninstr = 0
        self.dma_bufs = []

    def _wait(self, eng, ev):
        if ev is None:
            return
        sem, val = ev
        if eng == "pe" and sem is self.esem["pe"]:
            return
        key = id(sem)
        if self.waited[eng].get(key, 0) >= val:
            return
        self.waited[eng][key] = val
        self.prog[eng].append(("w", None, sem, val))

    def _deps(self, eng, reads, writes):
        for b in reads:
            self._wait(eng, b.w)
        for b in writes:
            self._wait(eng, b.w)
            for ev in list(b.r.values()):
                self._wait(eng, ev)

    def _commit(self, ev, reads, writes):
        sem, val = ev
        for b in reads:
            b.r[id(sem)] = ev
        for b in writes:
            b.w = ev
            b.r = {}

    def op(self, eng, fns, reads=(), writes=()):
        self._deps(eng, reads, writes)
        if not isinstance(fns, (list, tuple)):
            fns = [fns]
        calls = [f(REC) for f in fns]
        for c in calls[:-1]:
            self.prog[eng].append(("i", c, None, 0))
        self.ecnt[eng] += 1
        sem = self.esem[eng]
        self.prog[eng].append(("i", calls[-1], sem, 1))
        ev = (sem, self.ecnt[eng])
        self._commit(ev, reads, writes)
        self.ninstr += len(fns)
        return ev

    def dma(self, q, fn, reads=(), writes=(), sembuf=None):
        self._deps(q, reads, writes)
        b = sembuf
        if b.sem is None:
            b.sem = self.es.enter_context(self.nc.semaphore("ds%d" % self.nsem))
            self.nsem += 1
            self.dma_bufs.append(b)
        b.cnt += 16
        self.prog[q].append(("i", fn(REC), b.sem, 16))
        ev = (b.sem, b.cnt)
        self._commit(ev, reads, writes)
        return ev

    def replay(self, eng, e):
        for kind, c, sem, inc in self.prog[eng]:
            if kind == "w":
                e.wait_ge(sem, inc)
            else:
                name, a, k = c
                ins = getattr(e, name)(*a, **k)
                if sem is not None:
                    ins.then_inc(sem, inc)

    def barrier(self):
        for e in self.ALL:
            for x in self.CE:
                if self.ecnt[x] > 0:
                    self._wait(e, (self.esem[x], self.ecnt[x]))
        for b in self.dma_bufs:
            for e in ("pe", "act", "dve"):
                self._wait(e, (b.sem, b.cnt))


def build_nc(NU):
    nc = bass.Bass("TRN2", target_bir_lowering=False)
    es_outer = ExitStack()
    with es_outer as es:
        S = Sched(nc, es)

        def din(name, shape):
            return nc.dram_tensor(name, list(shape), F32, kind="ExternalInput").ap()

        x_d = din("x", [NU, EXT, D])
        kb_d = din("kbias", [128, NU * NVT_ALL])
        win_d = din("w_in", [D, IN_COLS])
        g1_d = din("norm_mix_g", [128, 8])
        bg_d = din("b_gate", [128, 16])
        cw_d = din("conv_w", [128, 18])
        wao_d = din("w_attn_out", [256, D])
        wco_d = din("w_conv_out", [768, D])
        wout_d = din("w_out", [D, D])
        g2_d = din("norm_ffn_g", [128, 8])
        wrg_d = din("w_route_group", [D, 4])
        brg_d = din("b_route_group", [4])
        wre_d = din("w_route_expert", [D, NE])
        bre_d = din("b_route_expert", [NE])
        w1_d = din("w1", [NE, D, FF])
        w3_d = din("w3", [NE, D, FF])
        w2_d = din("w2", [NE, FF, D])
        gf_d = din("norm_final_g", [D])
        ident_d = din("ident", [128, 128])
        abias_d = din("abias", [128, 3 * 2 * 4 * 128])
        shift_d = din("shiftI", [128, 128])
        out_d = nc.dram_tensor("out", [NU * U, D], F32, kind="ExternalOutput").ap()

        def sb(name, shape, dt):
            return es.enter_context(nc.sbuf_tensor(name, list(shape), dt))

        ident_bf = sb("ident_bf", [128, 128], BF16)
        ones_bf = sb("ones_bf", [128, 64], BF16)
        shiftI = sb("shiftI_sb", [128, 128], F32)
        g1col = sb("g1col", [128, 8], F32)
        g2col = sb("g2col", [128, 8], F32)
        bgate = sb("bgate", [128, 16], F32)
        cwcol = sb("cwcol", [128, 6, 3], F32)
        br_bc = sb("br_bc", [128, 36], F32)
        kbias = sb("kbias_sb", [128, NU * NVT_ALL], F32)
        stat_a = sb("stat_a", [128, 64], F32)
        stat_b = sb("stat_b", [128, 64], F32)
        stat_c = sb("stat_c", [128, 64], F32)
        wr_bf = sb("wr_bf", [128, 8, 36], BF16)
        mhalf = sb("mhalf", [128, 1], F32)
        b_const = Buf("const")
        b_constp = Buf("constp")
        b_stat = [Buf("stat%d" % i) for i in range(64)]

        remaining = nc.sbuf_bytes_remaining
        ARENA = 204 * 1024 + 512
        assert remaining >= ARENA, remaining
        arena = sb("arena", [128, ARENA // 4], F32)

        def view(off, shape, dt):
            n = 1
            for s_ in shape[1:]:
                n *= s_
            esz = 4 if dt == F32 else 2
            nb = n * esz
            assert off % 4 == 0 and nb % 4 == 0 and off + nb <= ARENA, (off, nb)
            ap = arena[:, off // 4:(off + nb) // 4]
            if dt != F32:
                ap = ap.bitcast(dt)
            if len(shape) == 3:
                ap = ap.rearrange("p (a b) -> p a b", a=shape[1])
            elif len(shape) == 4:
                ap = ap.rearrange("p (a b c) -> p a b c", a=shape[1], b=shape[2])
            return ap

        KB = 1024
        banks = [es.enter_context(nc.psum_tensor("bank%d" % i, [128, 512], F32)) for i in range(8)]
        pb = [Buf("bank%d" % i) for i in range(8)]

        def bank_bf(i):
            return banks[i][:, :].bitcast(BF16).rearrange("p (c n) -> p c n", c=8)

        S.dma("pool", lambda e: e.dma_start(out=ident_bf[:, :], in_=ident_d[:, :]), writes=[b_constp], sembuf=b_constp)
        S.dma("sp", lambda e: e.dma_start(out=shiftI[:, :], in_=shift_d[:, :]), writes=[b_const], sembuf=b_const)
        S.dma("sp", lambda e: e.dma_start(out=g1col[:, :], in_=g1_d[:, :]), writes=[b_const], sembuf=b_const)
        S.dma("sp", lambda e: e.dma_start(out=g2col[:, :], in_=g2_d[:, :]), writes=[b_const], sembuf=b_const)
        S.dma("sp", lambda e: e.dma_start(out=bgate[:, :], in_=bg_d[:, :]), writes=[b_const], sembuf=b_const)
        S.dma("sp", lambda e: e.dma_start(out=cwcol[:, :, :], in_=cw_d.rearrange("p (c k) -> p c k", k=3)), writes=[b_const], sembuf=b_const)
        S.dma("sp", lambda e: e.dma_start(out=br_bc[:, 0:4], in_=brg_d.partition_broadcast(128)), writes=[b_const], sembuf=b_const)
        S.dma("sp", lambda e: e.dma_start(out=br_bc[:, 4:36], in_=bre_d.partition_broadcast(128)), writes=[b_const], sembuf=b_const)
        S.dma("sp", lambda e: e.dma_start(out=kbias[:, :], in_=kb_d[:, :]), writes=[b_const], sembuf=b_const)
        S.dma("pool", lambda e: e.dma_start(out=wr_bf[:, :, 0:4], in_=wrg_d.rearrange("(c p) n -> p c n", p=128)), writes=[b_constp], sembuf=b_constp)
        S.dma("pool", lambda e: e.dma_start(out=wr_bf[:, :, 4:36], in_=wre_d.rearrange("(c p) n -> p c n", p=128)), writes=[b_constp], sembuf=b_constp)
        S.op("dve", [lambda e: e.memset(ones_bf[:, :], 1.0), lambda e: e.memset(mhalf[:, :], -0.5)], writes=[b_const])

        def norm_sq(src_ap, src_bufs, sq_ap, sq_buf, si):
            st = b_stat[si % 64]
            ca = stat_a[:, si % 64:si % 64 + 1]
            S.op("act", lambda e: e.activation(out=sq_ap, in_=src_ap, func=AF.Square, accum_out=ca),
                 reads=src_bufs, writes=[sq_buf, st])

        def norm_rstd(si):
            st = b_stat[si % 64]
            ca = stat_a[:, si % 64:si % 64 + 1]
            cb = stat_b[:, si % 64:si % 64 + 1]
            cc = stat_c[:, si % 64:si % 64 + 1]
            S.op("pool", lambda e: e.tensor_scalar(out=cb, in0=ca, scalar1=1.0 / D, scalar2=1e-6, op0=ALU.mult, op1=ALU.add),
                 reads=[], writes=[st])
            S.op("pool", lambda e: e.tensor_tensor(out=cc, in0=cb, in1=mhalf[:, 0:1], op=ALU.pow), reads=[b_const], writes=[st])
            return cc, st

        def norm_fin(src_ap, src_bufs, gcol, dst_ap, dst_buf, xn_ap, xn_buf, si, pbank):
            st = b_stat[si % 64]
            cc = stat_c[:, si % 64:si % 64 + 1]
            S.op("act", lambda e: e.activation(out=xn_ap, in_=src_ap, func=AF.Identity, scale=cc),
                 reads=src_bufs + [st], writes=[xn_buf])
            pt = bank_bf(pbank)
            S.op("pe", [lambda e, c=c: e.transpose(out=pt[:, c, :], in_=xn_ap[:, c * 128:(c + 1) * 128], identity=ident_bf[:, :])
                        for c in range(8)], reads=[xn_buf, b_constp], writes=[pb[pbank]])
            S.op("dve", lambda e: e.tensor_tensor(out=dst_ap, in0=pt[:, :, :], in1=gcol[:, :].unsqueeze(2).to_broadcast([128, 8, 128]), op=ALU.mult),
                 reads=[pb[pbank], b_const], writes=[dst_buf])

        wcols = lambda c0, n: win_d[:, c0:c0 + n].rearrange("(c p) n -> p c n", p=128)

        b_ost = []
        for u in range(NU):
            try:
                S.barrier()
                if STOP_PHASE == 0:
                    raise _Stop()
                hT = view(0, [128, 8, EXT], BF16)
                b_hT = [Buf("hT%d" % t) for t in range(8)]
                xts = [view(64 * KB + i * 4 * KB, [128, D], F32) for i in range(4)]
                b_xt = [Buf("xt%d" % i) for i in range(4)]
                xns = [view(80 * KB + i * 2 * KB, [128, D], BF16) for i in range(2)]
                b_xn = [Buf("xn%d" % i) for i in range(2)]
                sq = view(84 * KB, [128, D], BF16)
                b_sq = Buf("sq")
                def p1_dma(t):
                    S.dma("sp", lambda e: e.dma_start(out=xts[t % 4], in_=x_d[u, t * 128:(t + 1) * 128, :]),
                          writes=[b_xt[t % 4]], sembuf=b_xt[t % 4])
                for t in range(3):
                    p1_dma(t)
                for t in range(2):
                    norm_sq(xts[t], [b_xt[t]], sq, b_sq, t)
                    norm_rstd(t)
                for t in range(NT_EXT):
                    if t + 3 < NT_EXT:
                        p1_dma(t + 3)
                    if t + 2 < NT_EXT:
                        norm_sq(xts[(t + 2) % 4], [b_xt[(t + 2) % 4]], sq, b_sq, t + 2)
                        norm_rstd(t + 2)
                    norm_fin(xts[t % 4], [b_xt[t % 4]], g1col, hT[:, :, t * 128:(t + 1) * 128], b_hT[t // 4],
                             xns[t % 2], b_xn[t % 2], t, t % 2)
                S.barrier()
                if STOP_PHASE == 1:
                    raise _Stop()

                A1 = 64 * KB
                q_sb = view(A1, [128, 2, 2, U], BF16)
                k_sb = view(A1 + 16 * KB, [128, 2, EXT], BF16)
                v_sb = view(A1 + 32 * KB, [128, 32, 256], BF16)
                wqkv = [view(A1 + 48 * KB, [128, 8, 3, 256], BF16) for i in range(2)]
                b_q, b_k, b_v = Buf("q"), Buf("k"), Buf("v")
                _bw = [Buf("wqkv_%d" % j) for j in range(3)]
                b_wqkv = [_bw, _bw]
                S.op("dve", [lambda e: e.memset(q_sb[64:128, :, 0, :], 0.0), lambda e: e.memset(q_sb[0:64, :, 1, :], 0.0)], writes=[b_q])
                acc_od = view(128 * KB, [128, 4, U], F32)
                b_acc_od = Buf("acc_od")
                A3 = 160 * KB
                NSL = 4
                pTs = [view(A3 + i * KB, [128, 512], BF16) for i in range(NSL)]
                Ssb = [view(A3 + 4 * KB + i * 2 * KB, [128, 512], F32) for i in range(NSL)]
                ab_sb = view(A3 + 12 * KB, [128, 6, 512], F32)
                b_pT = [Buf("pT%d" % i) for i in range(NSL)]
                b_Ssb = [Buf("Ssb%d" % i) for i in range(NSL)]
                b_ab = Buf("abias")
                S.dma("sp", lambda e: e.dma_start(out=ab_sb, in_=abias_d.rearrange("p (a b) -> p a b", a=6)), writes=[b_ab], sembuf=b_ab)

                def load_qkv(g):
                    ws = wqkv[g % 2]
                    for j in range(3):
                        S.dma("pool", lambda e, j=j, ws=ws, g=g: e.dma_start(out=ws[:, :, j, :], in_=wcols(768 * j + 256 * g, 256)),
                              writes=[b_wqkv[g % 2][j]], sembuf=b_wqkv[g % 2][j])

                load_qkv(0)
                pbi = 0
                evac_i = 0
                for g in range(3):
                    dil = DIL[g]
                    ws = wqkv[g % 2]
                    bw = b_wqkv[g % 2]
                    kblocks = list(range(8)) if g == 2 else list(range(1, 7))
                    kbase = kblocks[0] * 512

                    def evac(dst, src, reads, writes):
                        nonlocal evac_i
                        evac_i += 1
                        if evac_i % 2 == 0:
                            S.op("act", lambda e: e.copy(out=dst, in_=src), reads=reads, writes=writes)
                        else:
                            S.op("dve", lambda e: e.tensor_copy(out=dst, in_=src), reads=reads, writes=writes)

                    for c in range(2):
                        for tb in range(4):
                            bk = pbi % 4
                            pbi += 1
                            t0 = HALO + tb * 512
                            S.op("pe", [lambda e, dc=dc, bk=bk, c=c, t0=t0: e.matmul(banks[bk][:, :], lhsT=ws[:, dc, 0, c * 128:(c + 1) * 128],
                                                                                  rhs=hT[:, dc, t0:t0 + 512], start=(dc == 0), stop=(dc == 7))
                                        for dc in range(8)], reads=[bw[0], b_hT[t0 // 512]], writes=[pb[bk]])
                            S.op("act", lambda e: e.copy(out=q_sb[0:64, c, 0, tb * 512:(tb + 1) * 512], in_=banks[bk][0:64, :]), reads=[pb[bk]], writes=[b_q])
                            S.op("dve", lambda e: e.tensor_copy(out=q_sb[64:128, c, 1, tb * 512:(tb + 1) * 512], in_=banks[bk][64:128, :]), reads=[pb[bk]], writes=[b_q])
                    if SUBSTOP == 'A':
                        raise _Stop()
                    for c in range(2):
                        for kbk in kblocks:
                            bk = pbi % 4
                            pbi += 1
                            t0 = kbk * 512
                            S.op("pe", [lambda e, dc=dc, bk=bk, c=c, t0=t0: e.matmul(banks[bk][:, :], lhsT=ws[:, dc, 1, c * 128:(c + 1) * 128],
                                                                                  rhs=hT[:, dc, t0:t0 + 512], start=(dc == 0), stop=(dc == 7))
                                        for dc in range(8)], reads=[bw[1], b_hT[kbk]], writes=[pb[bk]])
                            evac(k_sb[:, c, t0 - kbase:t0 - kbase + 512], banks[bk][:, :], [pb[bk]], [b_k])
                    if SUBSTOP == 'B':
                        raise _Stop()
                    nkt = NVT[g] // dil
                    vlist = [(r, kt) for r in range(dil) for kt in range(nkt)]
                    for i0 in range(0, len(vlist), 2):
                        bk = pbi % 4
                        pbi += 1
                        pair = vlist[i0:i0 + 2]
                        fns = []
                        for pi, (r, kt) in enumerate(pair):
                            s0 = HALO + r + dil * (128 * kt - 64)
                            for dc in range(8):
                                fns.append(lambda e, dc=dc, bk=bk, pi=pi, s0=s0: e.matmul(
                                    banks[bk][:, pi * 256:(pi + 1) * 256], lhsT=hT[:, dc, sl_(s0, dil)],
                                    rhs=ws[:, dc, 2, :], start=(dc == 0), stop=(dc == 7)))
                        S.op("pe", fns, reads=[bw[2]] + b_hT, writes=[pb[bk]])
                        vl0 = vt_local(g, pair[0][0], pair[0][1])
                        n = len(pair)
                        evac(v_sb[:, vl0:vl0 + n, :], banks[bk][:, 0:256 * n].rearrange("p (a b) -> p a b", a=n), [pb[bk]], [b_v])

                    if SUBSTOP == 'C':
                        raise _Stop()
                    if g + 1 < 3:
                        load_qkv(g + 1)
                    nq = U // dil // 128
                    steps = [(r, j, kk) for r in range(dil) for j in range(nq) for kk in range(2)]

                    def emit_S(i):
                        r, j, kk = steps[i]
                        kt = j + kk
                        bk = 2 + (i % NSL)
                        ks = HALO + r + dil * (128 * kt - 64) - kbase
                        qs = r + dil * 128 * j
                        fns = []
                        for h in range(4):
                            fns.append(lambda e, h=h, bk=bk, ks=ks, qs=qs: e.matmul(
                                banks[bk][:, h * 128:(h + 1) * 128], lhsT=k_sb[:, h // 2, sl_(ks, dil)],
                                rhs=q_sb[:, h // 2, h % 2, sl_(qs, dil)], start=True, stop=True))
                        S.op("pe", fns, reads=[b_k, b_q], writes=[pb[bk]])
                        sl = i % NSL
                        vt = VT_OFF[g] + vt_local(g, r, kt)
                        kcol = kbias[:, u * NVT_ALL + vt:u * NVT_ALL + vt + 1]
                        S.op("dve", lambda e: e.tensor_tensor(out=Ssb[sl], in0=banks[bk][:, :], in1=ab_sb[:, g * 2 + kk, :], op=ALU.add),
                             reads=[pb[bk], b_ab], writes=[b_Ssb[sl]])
                        S.op("act", lambda e: e.activation(out=pTs[sl], in_=Ssb[sl], func=AF.Exp, bias=kcol, scale=0.125),
                             reads=[b_Ssb[sl], b_const], writes=[b_pT[sl]])

                    def emit_rest(i):
                        r, j, kk = steps[i]
                        kt = j + kk
                        sl = i % NSL
                        vl = vt_local(g, r, kt)
                        nb = 6 + ((i // 2) % 2)
                        fns = []
                        for h in range(4):
                            fns.append(lambda e, h=h, nb=nb: e.matmul(
                                banks[nb][0:64, h * 128:(h + 1) * 128], lhsT=v_sb[:, vl, h * 64:(h + 1) * 64],
                                rhs=pTs[sl][:, h * 128:(h + 1) * 128], start=(kk == 0 and h == 0), stop=(kk == 1),
                                skip_group_check=True, tile_position=(0, 0)))
                        fns.append(lambda e, nb=nb: e.matmul(banks[nb][64:128, :], lhsT=ones_bf[:, :], rhs=pTs[sl][:, :],
                                                             start=(kk == 0), stop=(kk == 1), skip_group_check=True, tile_position=(0, 64)))
                        S.op("pe", fns, reads=[b_pT[sl], b_v, b_const], writes=[pb[nb]])
                        if kk == 1:
                            qs = r + dil * 128 * j
                            dst = acc_od[:, :, sl_(qs, dil)]
                            src = banks[nb][:, :].rearrange("p (a b) -> p a b", a=4)
                            if g == 0:
                                S.op("dve", lambda e: e.tensor_copy(out=dst, in_=src), reads=[pb[nb]], writes=[b_acc_od])
                            else:
                                S.op("dve", lambda e: e.tensor_tensor(out=dst, in0=dst, in1=src, op=ALU.add), reads=[pb[nb]], writes=[b_acc_od])

                    emit_S(0)
                    emit_S(1)
                    emit_S(2)
                    if SUBSTOP == 'D':
                        raise _Stop()
                    for i in range(len(steps)):
                        if i + 3 < len(steps):
                            emit_S(i + 3)
                        emit_rest(i)
                        if SUBSTOP == 'E' and i == 1:
                            raise _Stop()
                    if SUBSTOP == 'F':
                        raise _Stop()
                S.barrier()
                if STOP_PHASE == 2:
                    raise _Stop()

                attn_T = view(A3, [128, 4, U], BF16)
                ybT = view(A3 + 16 * KB, [128, 6, U], BF16)
                b_attn, b_yb = Buf("attn_T"), Buf("ybT")
                S.op("dve", lambda e: e.memset(attn_T[64:128, :, :], 0.0), writes=[b_attn])
                for tb in range(4):
                    blk = slice(tb * 512, (tb + 1) * 512)
                    S.op("act", lambda e, blk=blk: e.activation(out=acc_od[64:128, :, blk], in_=acc_od[64:128, :, blk], func=AF.Ln), writes=[b_acc_od])
                    S.op("act", lambda e, blk=blk: e.activation(out=acc_od[64:128, :, blk], in_=acc_od[64:128, :, blk], func=AF.Exp, scale=-1.0), writes=[b_acc_od])
                    for h in range(4):
                        bk = (tb * 4 + h) % 4
                        S.op("pe", lambda e, h=h, blk=blk, bk=bk: e.matmul(banks[bk][:, :], lhsT=shiftI[:, :], rhs=acc_od[:, h, blk],
                                                                         start=True, stop=True),
                             reads=[b_acc_od, b_const], writes=[pb[bk]])
                        S.op("dve", lambda e, h=h, blk=blk, bk=bk: e.tensor_tensor(out=attn_T[0:64, h, blk], in0=acc_od[0:64, h, blk],
                                                                                 in1=banks[bk][0:64, :], op=ALU.mult),
                             reads=[pb[bk], b_acc_od], writes=[b_attn])

                wcv = [view(A1 + i * 6 * KB, [128, 8, 3, 128], BF16) for i in range(2)]
                b_wcv = [[Buf("wcv%d_%d" % (i, j)) for j in range(3)] for i in range(2)]
                cg_sb = [view(A1 + 12 * KB + i * 2064, [128, 516], F32) for i in range(2)]
                u_sb = [view(A1 + 20 * KB + i * 2064, [128, 516], F32) for i in range(2)]
                c1_sb = [view(A1 + 28 * KB + i * 2 * KB, [128, 512], F32) for i in range(2)]
                b_cg = [Buf("cg%d" % i) for i in range(2)]
                b_u = [Buf("u%d" % i) for i in range(2)]
                b_c1 = [Buf("c1_%d" % i) for i in range(2)]

                def load_wcv(c):
                    for j in range(3):
                        S.dma("pool", lambda e, j=j, c=c: e.dma_start(out=wcv[c % 2][:, :, j, :], in_=wcols(2304 + 768 * j + 128 * c, 128)),
                              writes=[b_wcv[c % 2][j]], sembuf=b_wcv[c % 2][j])

                load_wcv(0)
                it = 0
                for c in range(6):
                    if c + 1 < 6:
                        load_wcv(c + 1)
                    wv_ = wcv[c % 2]
                    bwv = b_wcv[c % 2]
                    for tb in range(4):
                        sl = it % 2
                        bA, bB, bC, bD = [4 * sl + i for i in range(4)]
                        it += 1
                        t0 = HALO + tb * 512
                        hb = [b_hT[t0 // 512 - 1], b_hT[t0 // 512], b_hT[t0 // 512 + 1]]

                        def mm(bk, j, rhs_of, ncols, c0=0):
                            return [lambda e, dc=dc: e.matmul(banks[bk][:, c0:c0 + ncols], lhsT=wv_[:, dc, j, :], rhs=rhs_of(dc),
                                                              start=(dc == 0), stop=(dc == 7)) for dc in range(8)]
                        S.op("pe", mm(bA, 1, lambda dc: hT[:, dc, t0:t0 + 512], 512), reads=[bwv[1]] + hb, writes=[pb[bA]])
                        S.op("pe", mm(bB, 2, lambda dc: hT[:, dc, t0:t0 + 512], 512), reads=[bwv[2]] + hb, writes=[pb[bB]])
                        S.op("pe", mm(bC, 0, lambda dc: hT[:, dc, t0:t0 + 512], 512), reads=[bwv[0]] + hb, writes=[pb[bC]])
                        S.op("pe", mm(bD, 1, lambda dc: hT[:, dc, t0 - 1:t0 + 513:513], 2, 0)
                             + mm(bD, 2, lambda dc: hT[:, dc, t0 - 1:t0 + 513:513], 2, 2), reads=[bwv[1], bwv[2]] + hb, writes=[pb[bD]])
                        cg, uu, c1 = cg_sb[sl], u_sb[sl], c1_sb[sl]
                        S.op("act", [lambda e: e.copy(out=cg[:, 0:512], in_=banks[bA][:, :]),
                                     lambda e: e.copy(out=cg[:, 512:514], in_=banks[bD][:, 0:2])],
                             reads=[pb[bA], pb[bD]], writes=[b_cg[sl]])
                        S.op("dve", [lambda e: e.tensor_tensor(out=uu[:, 1:513], in0=cg[:, 0:512], in1=banks[bB][:, :], op=ALU.mult),
                                     lambda e: e.tensor_tensor(out=uu[:, 0:514:513], in0=cg[:, 512:514], in1=banks[bD][:, 2:4], op=ALU.mult)],
                             reads=[b_cg[sl], pb[bB], pb[bD]], writes=[b_u[sl]])
                        S.op("dve", lambda e: e.tensor_scalar(out=c1, in0=uu[:, 0:512], scalar1=cwcol[:, c, 0:1], scalar2=None, op0=ALU.mult),
                             reads=[b_u[sl], b_const], writes=[b_c1[sl]])
                        S.op("dve", lambda e: e.scalar_tensor_tensor(out=c1, in0=uu[:, 1:513], scalar=cwcol[:, c, 1:2], in1=c1, op0=ALU.mult, op1=ALU.add),
                             reads=[b_u[sl]], writes=[b_c1[sl]])
                        S.op("dve", lambda e: e.scalar_tensor_tensor(out=c1, in0=uu[:, 2:514], scalar=cwcol[:, c, 2:3], in1=c1, op0=ALU.mult, op1=ALU.add),
                             reads=[b_u[sl]], writes=[b_c1[sl]])
                        S.op("dve", lambda e: e.tensor_tensor(out=ybT[:, c, tb * 512:(tb + 1) * 512], in0=c1, in1=banks[bC][:, :], op=ALU.mult),
                             reads=[b_c1[sl], pb[bC]], writes=[b_yb])
                S.barrier()
                if STOP_PHASE == 3:
                    raise _Stop()

                acc = view(64 * KB, [128, NT_OWN, D], F32)
                b_acc = [Buf("acc%d" % i) for i in range(NT_OWN)]
                h2T = view(128 * KB, [128, 8, U], BF16)
                b_h2T = [Buf("h2T%d" % i) for i in range(4)]
                logits = view(200 * KB, [128, NT_OWN, 36], F32)
                comb = view(200 * KB + 2304, [128, NT_OWN, NE], F32)
                b_log, b_comb = Buf("logits"), Buf("comb")

                def mcol(tok):
                    return tok if tok < 1024 else 3072 + (tok - 1024)
                b_mrg = [Buf("mrg%d" % i) for i in range(4)]
                wg_b = [view(A1 + i * 4 * KB, [128, 8, 2, 128], BF16) for i in range(2)]
                wao_b = [view(A1 + 8 * KB + i * KB, [128, 4, 128], BF16) for i in range(2)]
                wco_b = [view(A1 + 10 * KB + i * 1536, [128, 6, 128], BF16) for i in range(2)]
                gts = [view(A1 + 14 * KB + i * 2 * KB, [128, 512], F32) for i in range(4)]
                b_bun = [[Buf("bun%d_%d" % (i, j)) for j in range(4)] for i in range(2)]
                b_gt = [Buf("gt%d" % i) for i in range(4)]

                def load_bundle(dcol):
                    sl = dcol % 2
                    S.dma("pool", lambda e: e.dma_start(out=wg_b[sl][:, :, 0, :], in_=wcols(4608 + dcol * 128, 128)), writes=[b_bun[sl][0]], sembuf=b_bun[sl][0])
                    S.dma("pool", lambda e: e.dma_start(out=wg_b[sl][:, :, 1, :], in_=wcols(4608 + D + dcol * 128, 128)), writes=[b_bun[sl][1]], sembuf=b_bun[sl][1])
                    S.dma("pool", lambda e: e.dma_start(out=wao_b[sl][0:64, :, :], in_=wao_d[:, dcol * 128:(dcol + 1) * 128].rearrange("(h e) n -> e h n", e=64)),
                          writes=[b_bun[sl][2]], sembuf=b_bun[sl][2])
                    S.dma("pool", lambda e: e.dma_start(out=wco_b[sl][:, :, :], in_=wco_d[:, dcol * 128:(dcol + 1) * 128].rearrange("(c p) n -> p c n", p=128)),
                          writes=[b_bun[sl][3]], sembuf=b_bun[sl][3])

                for _i in range(2):
                    S.op("dve", lambda e, _i=_i: e.memset(wao_b[_i][64:128, :, :], 0.0), writes=[b_bun[_i][2]])
                load_bundle(0)
                it4 = 0
                for dcol in range(8):
                    sl = dcol % 2
                    if dcol + 1 < 8:
                        load_bundle(dcol + 1)
                    for tb in range(4):
                        blk = slice(tb * 512, (tb + 1) * 512)
                        oblk = slice(HALO + tb * 512, HALO + (tb + 1) * 512)
                        mc = mcol(tb * 512)
                        ps = 4 * (it4 % 2)
                        gs = 2 * (it4 % 2)
                        it4 += 1
                        bYA, bYB, bG0, bG1 = ps, ps + 1, ps + 2, ps + 3
                        S.op("pe", [lambda e, h=h: e.matmul(banks[bYA][:, :], lhsT=wao_b[sl][:, h, :], rhs=attn_T[:, h, blk],
                                                            start=(h == 0), stop=(h == 3)) for h in range(4)],
                             reads=[b_bun[sl][2], b_attn], writes=[pb[bYA]])
                        S.op("pe", [lambda e, c=c: e.matmul(banks[bYB][:, :], lhsT=wco_b[sl][:, c, :], rhs=ybT[:, c, blk],
                                                            start=(c == 0), stop=(c == 5)) for c in range(6)],
                             reads=[b_bun[sl][3], b_yb], writes=[pb[bYB]])
                        S.op("pe", [lambda e, dc=dc: e.matmul(banks[bG0][:, :], lhsT=wg_b[sl][:, dc, 0, :], rhs=hT[:, dc, oblk],
                                                              start=(dc == 0), stop=(dc == 7)) for dc in range(8)],
                             reads=[b_bun[sl][0], b_hT[2 + tb]], writes=[pb[bG0]])
                        S.op("pe", [lambda e, dc=dc: e.matmul(banks[bG1][:, :], lhsT=wg_b[sl][:, dc, 1, :], rhs=hT[:, dc, oblk],
                                                              start=(dc == 0), stop=(dc == 7)) for dc in range(8)],
                             reads=[b_bun[sl][1], b_hT[2 + tb]], writes=[pb[bG1]])
                        g0s, g1s = gts[gs], gts[gs + 1]
                        S.op("act", lambda e: e.activation(out=g0s, in_=banks[bG0][:, :], func=AF.Sigmoid, bias=bgate[:, dcol:dcol + 1]),
                             reads=[pb[bG0], b_const], writes=[b_gt[gs]])
                        S.op("act", lambda e: e.activation(out=g1s, in_=banks[bG1][:, :], func=AF.Sigmoid, bias=bgate[:, 8 + dcol:9 + dcol]),
                             reads=[pb[bG1], b_const], writes=[b_gt[gs + 1]])
                        S.op("dve", lambda e: e.tensor_tensor(out=g0s, in0=g0s, in1=banks[bYA][:, :], op=ALU.mult),
                             reads=[pb[bYA]], writes=[b_gt[gs]])
                        S.op("dve", lambda e: e.tensor_tensor(out=g1s, in0=g1s, in1=banks[bYB][:, :], op=ALU.mult),
                             reads=[pb[bYB]], writes=[b_gt[gs + 1]])
                        S.op("dve", lambda e: e.tensor_tensor(out=hT[:, dcol, mc:mc + 512], in0=g0s, in1=g1s, op=ALU.add),
                             reads=[b_gt[gs], b_gt[gs + 1]], writes=[b_mrg[tb]])
                S.barrier()
                wout_sb = view(A3, [128, 8, D], BF16)
                xts4 = [view(A3 + 16 * KB + i * 4 * KB, [128, D], F32) for i in range(4)]
                xns4 = [view(A3 + 32 * KB + i * 2 * KB, [128, D], BF16) for i in range(2)]
                sq4 = view(A3 + 36 * KB, [128, D], BF16)
                b_wout = Buf("wout")
                b_xt4 = [Buf("xt4_%d" % i) for i in range(4)]
                b_xn4 = [Buf("xn4_%d" % i) for i in range(2)]
                b_sq4 = Buf("sq4")
                S.dma("pool", lambda e: e.dma_start(out=wout_sb, in_=wout_d.rearrange("(c p) n -> p c n", p=128)), writes=[b_wout], sembuf=b_wout)

                def p4_wout(ot):
                    sl = ot % 4
                    mc = mcol(ot * 128)
                    S.dma("sp", lambda e: e.dma_start(out=xts4[sl], in_=x_d[u, HALO + ot * 128:HALO + (ot + 1) * 128, :]),
                          writes=[b_xt4[sl]], sembuf=b_xt4[sl])
                    for half in range(2):
                        bk = (ot % 2) * 2 + half
                        hs = slice(half * 512, (half + 1) * 512)
                        S.op("pe", [lambda e, dcol=dcol: e.matmul(banks[bk][:, :], lhsT=hT[:, dcol, mc:mc + 128],
                                                                  rhs=wout_sb[:, dcol, hs], start=(dcol == 0), stop=(dcol == 7))
                                    for dcol in range(8)], reads=[b_mrg[ot // 4], b_wout], writes=[pb[bk]])
                        S.op("dve", lambda e: e.tensor_tensor(out=acc[:, ot, hs], in0=banks[bk][:, :], in1=xts4[sl][:, hs], op=ALU.add),
                             reads=[pb[bk], b_xt4[sl]], writes=[b_acc[ot]])

                def p4_rest(ot):
                    tb = ot // 4
                    norm_fin(acc[:, ot, :], [b_acc[ot]], g2col, h2T[:, :, ot * 128:(ot + 1) * 128], b_h2T[tb],
                             xns4[ot % 2], b_xn4[ot % 2], ot, 4 + ot % 2)

                def p4_router(ot):
                    tb = ot // 4
                    bk = 6 + (ot % 2)
                    S.op("pe", [lambda e, dc=dc: e.matmul(banks[bk][:, 0:36], lhsT=h2T[:, dc, ot * 128:(ot + 1) * 128], rhs=wr_bf[:, dc, :],
                                                          start=(dc == 0), stop=(dc == 7)) for dc in range(8)],
                         reads=[b_h2T[tb], b_constp], writes=[pb[bk]])
                    S.op("dve", lambda e: e.tensor_tensor(out=logits[:, ot, :], in0=banks[bk][:, 0:36], in1=br_bc[:, :], op=ALU.add),
                         reads=[pb[bk], b_const], writes=[b_log])

                for ot in range(3):
                    p4_wout(ot)
                for ot in range(2):
                    norm_sq(acc[:, ot, :], [b_acc[ot]], sq4, b_sq4, ot)
                    norm_rstd(ot)
                for ot in range(NT_OWN):
                    if ot + 3 < NT_OWN:
                        p4_wout(ot + 3)
                    if ot + 2 < NT_OWN:
                        norm_sq(acc[:, ot + 2, :], [b_acc[ot + 2]], sq4, b_sq4, ot + 2)
                        norm_rstd(ot + 2)
                    p4_rest(ot)
                    if ot >= 1:
                        p4_router(ot - 1)
                p4_router(NT_OWN - 1)
                S.barrier()
                if STOP_PHASE == 4:
                    raise _Stop()

                T = NT_OWN
                R0 = 0
                def rv(off, shape):
                    return view(R0 + off, shape, F32)
                cmax = rv(0, [128, T])
                oh = rv(256, [128, T, 4])
                ctmp = rv(768, [128, T, 4])
                csum = rv(1280, [128, T])
                pg = rv(1536, [128, T])
                tmp4 = rv(2048, [128, T, 4, 8])
                fs = rv(4096, [128, T, 8])
                srt = rv(4608, [128, T, 8])
                m1 = rv(5120, [128, T, 8])
                m2 = rv(5632, [128, T, 8])
                dl = rv(6144, [128, T])
                sg = rv(6400, [128, T])
                w1v = rv(6656, [128, T])
                w2v = rv(6912, [128, T])
                wdv = rv(7168, [128, T])
                b_r = Buf("router")
                coarse = logits[:, :, 0:4]
                fine = logits[:, :, 4:36].rearrange("p t (g e) -> p t g e", g=4)
                RW = dict(reads=[b_log], writes=[b_r])
                S.op("dve", lambda e: e.tensor_reduce(out=cmax, in_=coarse, axis=AX.X, op=ALU.max), **RW)
                S.op("dve", lambda e: e.tensor_tensor(out=oh, in0=coarse, in1=cmax.unsqueeze(2).to_broadcast([128, T, 4]), op=ALU.is_equal), **RW)
                S.op("dve", lambda e: e.tensor_tensor(out=ctmp, in0=coarse, in1=cmax.unsqueeze(2).to_broadcast([128, T, 4]), op=ALU.subtract), **RW)
                S.op("act", lambda e: e.activation(out=ctmp, in_=ctmp, func=AF.Exp), **RW)
                S.op("dve", lambda e: e.tensor_reduce(out=csum, in_=ctmp, axis=AX.X, op=ALU.add), **RW)
                S.op("dve", lambda e: e.reciprocal(out=pg, in_=csum), **RW)
                S.op("dve", lambda e: e.tensor_tensor(out=tmp4, in0=fine, in1=oh.unsqueeze(3).to_broadcast([128, T, 4, 8]), op=ALU.mult), **RW)
                S.op("dve", lambda e: e.tensor_reduce(out=fs, in_=tmp4.rearrange("p t g e -> p t e g"), axis=AX.X, op=ALU.add), **RW)
                for t in range(T):
                    S.op("dve", lambda e, t=t: e.max(out=srt[:, t, :], in_=fs[:, t, :]), **RW)
                S.op("dve", lambda e: e.tensor_tensor(out=m1, in0=fs, in1=srt[:, :, 0:1].to_broadcast([128, T, 8]), op=ALU.is_equal), **RW)
                S.op("dve", lambda e: e.tensor_tensor(out=m2, in0=fs, in1=srt[:, :, 1:2].to_broadcast([128, T, 8]), op=ALU.is_ge), **RW)
                S.op("dve", lambda e: e.tensor_tensor(out=dl, in0=srt[:, :, 0], in1=srt[:, :, 1], op=ALU.subtract), **RW)
                S.op("act", lambda e: e.activation(out=sg, in_=dl, func=AF.Sigmoid), **RW)
                S.op("dve", lambda e: e.tensor_tensor(out=w1v, in0=pg, in1=sg, op=ALU.mult), **RW)
                S.op("dve", lambda e: e.tensor_tensor(out=w2v, in0=pg, in1=w1v, op=ALU.subtract), **RW)
                S.op("dve", lambda e: e.tensor_tensor(out=wdv, in0=w1v, in1=w2v, op=ALU.subtract), **RW)
                S.op("dve", lambda e: e.tensor_tensor(out=m1, in0=m1, in1=wdv.unsqueeze(2).to_broadcast([128, T, 8]), op=ALU.mult), **RW)
                S.op("dve", lambda e: e.tensor_tensor(out=m2, in0=m2, in1=w2v.unsqueeze(2).to_broadcast([128, T, 8]), op=ALU.mult), **RW)
                S.op("dve", lambda e: e.tensor_tensor(out=m1, in0=m1, in1=m2, op=ALU.add), **RW)
                S.op("dve", lambda e: e.tensor_tensor(out=comb.rearrange("p t (g e) -> p t g e", g=4),
                                                      in0=oh.unsqueeze(3).to_broadcast([128, T, 4, 8]),
                                                      in1=m1.unsqueeze(2).to_broadcast([128, T, 4, 8]), op=ALU.mult),
                     reads=[b_r], writes=[b_comb])
                S.barrier()
                if STOP_PHASE == 5:
                    raise _Stop()

                w1s = [view(i * 24 * KB, [128, 8, FF], BF16) for i in range(2)]
                w3s = [view(i * 24 * KB + 8 * KB, [128, 8, FF], BF16) for i in range(2)]
                w2s = [view(i * 24 * KB + 16 * KB, [128, 4, D], BF16) for i in range(2)]
                b_w = [[Buf("mw%d_%d" % (i, j)) for j in range(3)] for i in range(2)]
                hid = [view(48 * KB + i * 4 * KB, [128, 4, 512], BF16) for i in range(2)]
                b_hid = [Buf("hid%d" % i) for i in range(2)]
                sas = [view(56 * KB + i * 2 * KB, [128, 512], F32) for i in range(2)]
                b_sa = [Buf("sa%d" % i) for i in range(2)]

                def load_expert(ex):
                    sl = ex % 2
                    S.dma("pool", lambda e: e.dma_start(out=w1s[sl], in_=w1_d[ex].rearrange("(c p) n -> p c n", p=128)), writes=[b_w[sl][0]], sembuf=b_w[sl][0])
                    S.dma("pool", lambda e: e.dma_start(out=w3s[sl], in_=w3_d[ex].rearrange("(c p) n -> p c n", p=128)), writes=[b_w[sl][1]], sembuf=b_w[sl][1])
                    S.dma("pool", lambda e: e.dma_start(out=w2s[sl], in_=w2_d[ex].rearrange("(c p) n -> p c n", p=128)), writes=[b_w[sl][2]], sembuf=b_w[sl][2])

                msteps = [(ex, tb) for ex in range(NE) for tb in range(4)]
                sa_i = 0

                def emit_up(i):
                    nonlocal sa_i
                    ex, tb = msteps[i]
                    sl = ex % 2
                    hs = i % 2
                    blk = slice(tb * 512, (tb + 1) * 512)
                    for fc in range(4):
                        bA = 2 * (fc % 2)
                        bB = bA + 1
                        S.op("pe", [lambda e, dc=dc: e.matmul(banks[bA][:, :], lhsT=w1s[sl][:, dc, fc * 128:(fc + 1) * 128], rhs=h2T[:, dc, blk],
                                                              start=(dc == 0), stop=(dc == 7)) for dc in range(8)],
                             reads=[b_w[sl][0], b_h2T[tb]], writes=[pb[bA]])
                        S.op("pe", [lambda e, dc=dc: e.matmul(banks[bB][:, :], lhsT=w3s[sl][:, dc, fc * 128:(fc + 1) * 128], rhs=h2T[:, dc, blk],
                                                              start=(dc == 0), stop=(dc == 7)) for dc in range(8)],
                             reads=[b_w[sl][1], b_h2T[tb]], writes=[pb[bB]])
                        ss = sa_i % 2
                        sa_i += 1
                        S.op("act", lambda e: e.activation(out=sas[ss], in_=banks[bA][:, :], func=AF.Silu), reads=[pb[bA]], writes=[b_sa[ss]])
                        S.op("dve", lambda e: e.tensor_tensor(out=hid[hs][:, fc, :], in0=sas[ss], in1=banks[bB][:, :], op=ALU.mult),
                             reads=[b_sa[ss], pb[bB]], writes=[b_hid[hs]])

                def emit_down(i):
                    ex, tb = msteps[i]
                    sl = ex % 2
                    hs = i % 2
                    for tt in range(4):
                        ot = tb * 4 + tt
                        for half in range(2):
                            bk = 4 + (tt * 2 + half) % 4
                            cs = slice(half * 512, (half + 1) * 512)
                            S.op("pe", [lambda e, fc=fc: e.matmul(banks[bk][:, :], lhsT=hid[hs][:, fc, tt * 128:(tt + 1) * 128], rhs=w2s[sl][:, fc, cs],
                                                                  start=(fc == 0), stop=(fc == 3)) for fc in range(4)],
                                 reads=[b_hid[hs], b_w[sl][2]], writes=[pb[bk]])
                            S.op("dve", lambda e: e.scalar_tensor_tensor(out=acc[:, ot, cs], in0=banks[bk][:, :], scalar=comb[:, ot, ex:ex + 1],
                                                                         in1=acc[:, ot, cs], op0=ALU.mult, op1=ALU.add),
                                 reads=[pb[bk], b_comb], writes=[b_acc[ot]])

                load_expert(0)
                load_expert(1)
                emit_up(0)
                for i in range(len(msteps)):
                    if i + 1 < len(msteps):
                        emit_up(i + 1)
                    emit_down(i)
                    ex, tb = msteps[i]
                    if tb == 3 and ex + 2 < NE:
                        load_expert(ex + 2)
                S.barrier()
                if STOP_PHASE == 6:
                    raise _Stop()

                gF = view(0, [128, D], F32)
                b_gF = Buf("gF")
                ost = [view(4 * KB + i * 4 * KB, [128, D], F32) for i in range(4)]
                b_ost = [Buf("ost%d" % i) for i in range(4)]
                sq7 = view(20 * KB, [128, D], BF16)
                b_sq7 = Buf("sq7")
                S.dma("sp", lambda e: e.dma_start(out=gF, in_=gf_d.partition_broadcast(128)), writes=[b_gF], sembuf=b_gF)
                for ot in range(2):
                    norm_sq(acc[:, ot, :], [b_acc[ot]], sq7, b_sq7, ot)
                    norm_rstd(ot)
                for ot in range(NT_OWN):
                    sl = ot % 4
                    if ot + 2 < NT_OWN:
                        norm_sq(acc[:, ot + 2, :], [b_acc[ot + 2]], sq7, b_sq7, ot + 2)
                        norm_rstd(ot + 2)
                    st = b_stat[ot]
                    cc = stat_c[:, ot:ot + 1]
                    S.op("dve", lambda e: e.scalar_tensor_tensor(out=ost[sl], in0=acc[:, ot, :], scalar=cc, in1=gF, op0=ALU.mult, op1=ALU.mult),
                         reads=[b_acc[ot], st, b_gF], writes=[b_ost[sl]])
                    S.dma("sp", lambda e, ot=ot, sl=sl: e.dma_start(out=out_d[u * U + ot * 128:u * U + (ot + 1) * 128, :], in_=ost[sl]),
                          reads=[b_ost[sl]], sembuf=b_ost[sl])
                S.barrier()
                if STOP_PHASE == 7:
                    raise _Stop()
            except _Stop:
                pass
        for b in S.dma_bufs:
            S._wait("sp", (b.sem, b.cnt))
        for b in b_ost:
            for ev in list(b.r.values()):
                S._wait("sp", ev)

        block = es.enter_context(nc.Block())

        @block.tensor
        def _(e):
            S.replay("pe", e)

        @block.scalar
        def _(e):
            S.replay("act", e)

        @block.vector
        def _(e):
            S.replay("dve", e)

        @block.gpsimd
        def _(e):
            S.replay("pool", e)

        @block.sync
        def _(e):
            S.replay("sp", e)
    return nc


def alibi_tables():
    slopes = np.array([2.0 ** (-8.0 * (i + 1) / 12) for i in range(12)], dtype=np.float64).reshape(3, 4)
    kk_ = np.arange(128)[:, None]
    qq_ = np.arange(128)[None, :]
    tab = np.zeros((128, 3, 2, 4, 128), dtype=np.float32)
    for g in range(3):
        for kk in range(2):
            delta = kk_ - qq_ - 64 + 128 * kk
            valid = np.abs(delta) <= 64
            for h in range(4):
                b = -slopes[g, h] * np.abs(delta) * DIL[g] * 8.0
                tab[:, g, kk, h, :] = np.where(valid, b, MASKV * 8.0).astype(np.float32)
    return tab.reshape(128, -1)


def key_bias(unit_start, seq_len):
    kb = np.zeros((128, NVT_ALL), dtype=np.float32)
    p = np.arange(128)
    for g in range(3):
        dil = DIL[g]
        nkt = NVT[g] // dil
        for r in range(dil):
            for kt in range(nkt):
                pos = unit_start + r + dil * (128 * kt - 64 + p)
                ok = (pos >= 0) & (pos < seq_len)
                kb[:, VT_OFF[g] + vt_local(g, r, kt)] = np.where(ok, 0.0, MASKV)
    return kb


def make_core_inputs(x, weights, n_cores, NU, seq_len):
    B, Sx, _ = x.shape
    units_per_seq = Sx // U
    xp = np.zeros((B, Sx + 2 * HALO, D), dtype=np.float32)
    xp[:, HALO:HALO + Sx] = x
    shared = dict(weights)
    shared["norm_mix_g"] = np.ascontiguousarray(weights["norm_mix_g"].reshape(8, 128).T)
    shared["norm_ffn_g"] = np.ascontiguousarray(weights["norm_ffn_g"].reshape(8, 128).T)
    shared["b_gate"] = np.ascontiguousarray(weights["b_gate"].reshape(16, 128).T)
    shared["conv_w"] = np.ascontiguousarray(weights["conv_w"].reshape(3, 6, 128).transpose(2, 1, 0).reshape(128, 18))
    shared["ident"] = np.eye(128, dtype=np.float32)
    shared["abias"] = alibi_tables()
    sh = np.zeros((128, 128), dtype=np.float32)
    sh[64 + (np.arange(128) % 64), np.arange(128)] = 1.0
    shared["shiftI"] = sh
    maps = []
    for c in range(n_cores):
        xs = np.zeros((NU, EXT, D), dtype=np.float32)
        kb = np.zeros((128, NU * NVT_ALL), dtype=np.float32)
        for uu in range(NU):
            gu = c * NU + uu
            b, us = divmod(gu, units_per_seq)
            xs[uu] = xp[b, us * U:us * U + EXT]
            kb[:, uu * NVT_ALL:(uu + 1) * NVT_ALL] = key_bias(us * U, seq_len)
        m = dict(shared)
        m["x"] = xs
        m["kbias"] = kb
        maps.append(m)
    return maps


_NC_CACHE = {}


def run(x, weights, n_cores, NU):
    if NU not in _NC_CACHE:
        _NC_CACHE[NU] = build_nc(NU)
    nc = _NC_CACHE[NU]
    B, Sx, _ = x.shape
    maps = make_core_inputs(x, weights, n_cores, NU, Sx)
    res = run_bass_kernel_spmd(nc, maps, core_ids=list(range(n_cores)))
    outs = [r["out"] for r in res.results]
    return np.concatenate(outs, axis=0).reshape(B, Sx, D)


def kernel(x, norm_mix_g, w_in, b_gate, conv_w, w_attn_out, w_conv_out, w_out, norm_ffn_g,
           w_route_group, b_route_group, w_route_expert, b_route_expert, w1, w3, w2, norm_final_g):
    f = lambda a: np.ascontiguousarray(np.asarray(a, dtype=np.float32))
    weights = {
        "norm_mix_g": f(norm_mix_g)[0], "w_in": f(w_in)[0], "b_gate": f(b_gate)[0], "conv_w": f(conv_w)[0],
        "w_attn_out": f(w_attn_out)[0], "w_conv_out": f(w_conv_out)[0], "w_out": f(w_out)[0],
        "norm_ffn_g": f(norm_ffn_g)[0], "w_route_group": f(w_route_group)[0], "b_route_group": f(b_route_group)[0],
        "w_route_expert": f(w_route_expert)[0], "b_route_expert": f(b_route_expert)[0],
        "w1": f(w1)[0], "w3": f(w3)[0], "w2": f(w2)[0], "norm_final_g": f(norm_final_g),
    }
    x = f(x)
    return run(x, weights, N_CORES, 2).astype(np.float32)
```

```python
import numpy as np
from contextlib import ExitStack
import concourse.bass as bass
import concourse.mybir as mybir
from concourse.bass_utils import run_bass_kernel_spmd

F32 = mybir.dt.float32
BF16 = mybir.dt.bfloat16
AF = mybir.ActivationFunctionType
ALU = mybir.AluOpType
AX = mybir.AxisListType

D = 1024
U = 2048
HALO = 1024
EXT = U + 2 * HALO
NT_EXT = EXT // 128
NT_OWN = U // 128
DIL = (1, 4, 16)
NVT = (17, 20, 32)
VT_OFF = (0, 17, 37)
NVT_ALL = 69
NE = 32
FF = 512
IN_COLS = 6656
N_CORES = 8
SEQ = 8192
MASKV = -30000.0
STOP_PHASE = -1
SUBSTOP = ''


class _Stop(Exception):
    pass


def sl_(start, step, n=128):
    return slice(start, start + (n - 1) * step + 1, step)


def vt_local(g, r, kt):
    return r * (NVT[g] // DIL[g]) + kt


class _Rec:
    def __getattr__(self, name):
        def m(*a, **k):
            return (name, a, k)
        return m


REC = _Rec()


class Buf:
    __slots__ = ("name", "w", "r", "sem", "cnt")

    def __init__(self, name):
        self.name = name
        self.w = None
        self.r = {}
        self.sem = None
        self.cnt = 0


class Sched:
    CE = ("pe", "act", "dve", "pool")
    ALL = ("pe", "act", "dve", "pool", "sp")

    def __init__(self, nc, es):
        self.nc = nc
        self.es = es
        self.prog = {e: [] for e in self.ALL}
        self.esem = {e: es.enter_context(nc.semaphore("es_" + e)) for e in self.CE}
        self.ecnt = {e: 0 for e in self.CE}
        self.waited = {e: {} for e in self.ALL}
        self.nsem = 0
        self.ninstr = 0
        self.dma_bufs = []
        self.semcache = {}

    def _wait(self, eng, ev):
        if ev is None:
            return
        sem, val = ev
        if eng == "pe" and sem is self.esem["pe"]:
            return
        key = id(sem)
        if self.waited[eng].get(key, 0) >= val:
            return
        self.waited[eng][key] = val
        self.prog[eng].append(("w", None, sem, val))

    def _deps(self, eng, reads, writes):
        for b in reads:
            self._wait(eng, b.w)
        for b in writes:
            self._wait(eng, b.w)
            for ev in list(b.r.values()):
                self._wait(eng, ev)

    def _commit(self, ev, reads, writes):
        sem, val = ev
        for b in reads:
            b.r[id(sem)] = ev
        for b in writes:
            b.w = ev
            b.r = {}

    def op(self, eng, fns, reads=(), writes=()):
        self._deps(eng, reads, writes)
        if not isinstance(fns, (list, tuple)):
            fns = [fns]
        calls = [f(REC) for f in fns]
        for c in calls[:-1]:
            self.prog[eng].append(("i", c, None, 0))
        self.ecnt[eng] += 1
        sem = self.esem[eng]
        self.prog[eng].append(("i", calls[-1], sem, 1))
        ev = (sem, self.ecnt[eng])
        self._commit(ev, reads, writes)
        self.ninstr += len(fns)
        return ev

    def dma(self, q, fn, reads=(), writes=(), sembuf=None):
        self._deps(q, reads, writes)
        b = sembuf
        if b.sem is None:
            if b.name in self.semcache:
                old = self.semcache[b.name]
                b.sem, b.cnt = old.sem, old.cnt
                self.dma_bufs.remove(old)
            else:
                b.sem = self.es.enter_context(self.nc.semaphore("ds%d" % self.nsem))
                self.nsem += 1
            self.semcache[b.name] = b
            self.dma_bufs.append(b)
        b.cnt += 16
        self.prog[q].append(("i", fn(REC), b.sem, 16))
        ev = (b.sem, b.cnt)
        self._commit(ev, reads, writes)
        return ev

    def replay(self, eng, e):
        for kind, c, sem, inc in self.prog[eng]:
            if kind == "w":
                e.wait_ge(sem, inc)
            else:
                name, a, k = c
                ins = getattr(e, name)(*a, **k)
                if sem is not None:
                    ins.then_inc(sem, inc)

    def barrier(self):
        for e in self.ALL:
            for x in self.CE:
                if self.ecnt[x] > 0:
                    self._wait(e, (self.esem[x], self.ecnt[x]))
        for b in self.dma_bufs:
            for e in ("pe", "act", "dve"):
                self._wait(e, (b.sem, b.cnt))


def build_nc(NU):
    nc = bass.Bass("TRN2", target_bir_lowering=False)
    es_outer = ExitStack()
    with es_outer as es:
        S = Sched(nc, es)

        def din(name, shape):
            return nc.dram_tensor(name, list(shape), F32, kind="ExternalInput").ap()

        x_d = din("x", [NU, EXT, D])
        kb_d = din("kbias", [128, NU * NVT_ALL])
        win_d = din("w_in", [D, IN_COLS])
        g1_d = din("norm_mix_g", [128, 8])
        bg_d = din("b_gate", [128, 16])
        cw_d = din("conv_w", [128, 18])
        wao_d = din("w_attn_out", [256, D])
        wco_d = din("w_conv_out", [768, D])
        wout_d = din("w_out", [D, D])
        g2_d = din("norm_ffn_g", [128, 8])
        wrg_d = din("w_route_group", [D, 4])
        brg_d = din("b_route_group", [4])
        wre_d = din("w_route_expert", [D, NE])
        bre_d = din("b_route_expert", [NE])
        w1_d = din("w1", [NE, D, FF])
        w3_d = din("w3", [NE, D, FF])
        w2_d = din("w2", [NE, FF, D])
        gf_d = din("norm_final_g", [D])
        ident_d = din("ident", [128, 128])
        abias_d = din("abias", [128, 3 * 2 * 4 * 128])
        shift_d = din("shiftI", [128, 128])
        out_d = nc.dram_tensor("out", [NU * U, D], F32, kind="ExternalOutput").ap()

        def sb(name, shape, dt):
            return es.enter_context(nc.sbuf_tensor(name, list(shape), dt))

        ident_bf = sb("ident_bf", [128, 128], BF16)
        ones_bf = sb("ones_bf", [128, 64], BF16)
        shiftI = sb("shiftI_sb", [128, 128], F32)
        g1col = sb("g1col", [128, 8], F32)
        g2col = sb("g2col", [128, 8], F32)
        bgate = sb("bgate", [128, 16], F32)
        cwcol = sb("cwcol", [128, 6, 3], F32)
        br_bc = sb("br_bc", [128, 36], F32)
        kbias = sb("kbias_sb", [128, NU * NVT_ALL], F32)
        stat_a = sb("stat_a", [128, 64], F32)
        stat_b = sb("stat_b", [128, 64], F32)
        stat_c = sb("stat_c", [128, 64], F32)
        wr_bf = sb("wr_bf", [128, 8, 36], BF16)
        mhalf = sb("mhalf", [128, 1], F32)
        b_const = Buf("const")
        b_constp = Buf("constp")
        b_stat = [Buf("stat%d" % i) for i in range(64)]

        remaining = nc.sbuf_bytes_remaining
        ARENA = 204 * 1024 + 512
        assert remaining >= ARENA, remaining
        arena = sb("arena", [128, ARENA // 4], F32)

        def view(off, shape, dt):
            n = 1
            for s_ in shape[1:]:
                n *= s_
            esz = 4 if dt == F32 else 2
            nb = n * esz
            assert off % 4 == 0 and nb % 4 == 0 and off + nb <= ARENA, (off, nb)
            ap = arena[:, off // 4:(off + nb) // 4]
            if dt != F32:
                ap = ap.bitcast(dt)
            if len(shape) == 3:
                ap = ap.rearrange("p (a b) -> p a b", a=shape[1])
            elif len(shape) == 4:
                ap = ap.rearrange("p (a b c) -> p a b c", a=shape[1], b=shape[2])
            return ap

        KB = 1024
        banks = [es.enter_context(nc.psum_tensor("bank%d" % i, [128, 512], F32)) for i in range(8)]
        pb = [Buf("bank%d" % i) for i in range(8)]

        def bank_bf(i):
            return banks[i][:, :].bitcast(BF16).rearrange("p (c n) -> p c n", c=8)

        S.dma("pool", lambda e: e.dma_start(out=ident_bf[:, :], in_=ident_d[:, :]), writes=[b_constp], sembuf=b_constp)
        S.dma("sp", lambda e: e.dma_start(out=shiftI[:, :], in_=shift_d[:, :]), writes=[b_const], sembuf=b_const)
        S.dma("sp", lambda e: e.dma_start(out=g1col[:, :], in_=g1_d[:, :]), writes=[b_const], sembuf=b_const)
        S.dma("sp", lambda e: e.dma_start(out=g2col[:, :], in_=g2_d[:, :]), writes=[b_const], sembuf=b_const)
        S.dma("sp", lambda e: e.dma_start(out=bgate[:, :], in_=bg_d[:, :]), writes=[b_const], sembuf=b_const)
        S.dma("sp", lambda e: e.dma_start(out=cwcol[:, :, :], in_=cw_d.rearrange("p (c k) -> p c k", k=3)), writes=[b_const], sembuf=b_const)
        S.dma("sp", lambda e: e.dma_start(out=br_bc[:, 0:4], in_=brg_d.partition_broadcast(128)), writes=[b_const], sembuf=b_const)
        S.dma("sp", lambda e: e.dma_start(out=br_bc[:, 4:36], in_=bre_d.partition_broadcast(128)), writes=[b_const], sembuf=b_const)
        S.dma("sp", lambda e: e.dma_start(out=kbias[:, :], in_=kb_d[:, :]), writes=[b_const], sembuf=b_const)
        S.dma("pool", lambda e: e.dma_start(out=wr_bf[:, :, 0:4], in_=wrg_d.rearrange("(c p) n -> p c n", p=128)), writes=[b_constp], sembuf=b_constp)
        S.dma("pool", lambda e: e.dma_start(out=wr_bf[:, :, 4:36], in_=wre_d.rearrange("(c p) n -> p c n", p=128)), writes=[b_constp], sembuf=b_constp)
        S.op("dve", [lambda e: e.memset(ones_bf[:, :], 1.0), lambda e: e.memset(mhalf[:, :], -0.5)], writes=[b_const])

        def norm_sq(src_ap, src_bufs, sq_ap, sq_buf, si):
            st = b_stat[si % 64]
            ca = stat_a[:, si % 64:si % 64 + 1]
            S.op("act", lambda e: e.activation(out=sq_ap, in_=src_ap, func=AF.Square, accum_out=ca),
                 reads=src_bufs, writes=[sq_buf, st])

        def norm_rstd(si):
            st = b_stat[si % 64]
            ca = stat_a[:, si % 64:si % 64 + 1]
            cb = stat_b[:, si % 64:si % 64 + 1]
            cc = stat_c[:, si % 64:si % 64 + 1]
            S.op("pool", lambda e: e.tensor_scalar(out=cb, in0=ca, scalar1=1.0 / D, scalar2=1e-6, op0=ALU.mult, op1=ALU.add),
                 reads=[], writes=[st])
            S.op("pool", lambda e: e.tensor_tensor(out=cc, in0=cb, in1=mhalf[:, 0:1], op=ALU.pow), reads=[b_const], writes=[st])
            return cc, st

        def norm_fin(src_ap, src_bufs, gcol, dst_ap, dst_buf, xn_ap, xn_buf, si, pbank):
            st = b_stat[si % 64]
            cc = stat_c[:, si % 64:si % 64 + 1]
            S.op("act", lambda e: e.activation(out=xn_ap, in_=src_ap, func=AF.Identity, scale=cc),
                 reads=src_bufs + [st], writes=[xn_buf])
            pt = bank_bf(pbank)
            S.op("pe", [lambda e, c=c: e.transpose(out=pt[:, c, :], in_=xn_ap[:, c * 128:(c + 1) * 128], identity=ident_bf[:, :])
                        for c in range(8)], reads=[xn_buf, b_constp], writes=[pb[pbank]])
            S.op("dve", lambda e: e.tensor_tensor(out=dst_ap, in0=pt[:, :, :], in1=gcol[:, :].unsqueeze(2).to_broadcast([128, 8, 128]), op=ALU.mult),
                 reads=[pb[pbank], b_const], writes=[dst_buf])

        wcols = lambda c0, n: win_d[:, c0:c0 + n].rearrange("(c p) n -> p c n", p=128)

        b_ost = []
        for u in range(NU):
            try:
                S.barrier()
                if STOP_PHASE == 0:
                    raise _Stop()
                hT = view(0, [128, 8, EXT], BF16)
                b_hT = [Buf("hT%d" % t) for t in range(8)]
                NXS = 8
                xts = [view(64 * KB + i * 4 * KB, [128, D], F32) for i in range(NXS)]
                b_xt = [Buf("xt%d" % i) for i in range(NXS)]
                xns = [view(96 * KB + i * 2 * KB, [128, D], BF16) for i in range(2)]
                b_xn = [Buf("xn%d" % i) for i in range(2)]
                sq = view(100 * KB, [128, D], BF16)
                b_sq = Buf("sq")
                def p1_dma(t):
                    S.dma("sp", lambda e: e.dma_start(out=xts[t % NXS], in_=x_d[u, t * 128:(t + 1) * 128, :]),
                          writes=[b_xt[t % NXS]], sembuf=b_xt[t % NXS])
                for t in range(NXS - 1):
                    p1_dma(t)
                for t in range(2):
                    norm_sq(xts[t], [b_xt[t]], sq, b_sq, t)
                    norm_rstd(t)
                for t in range(NT_EXT):
                    if t + NXS - 1 < NT_EXT:
                        p1_dma(t + NXS - 1)
                    if t + 2 < NT_EXT:
                        norm_sq(xts[(t + 2) % NXS], [b_xt[(t + 2) % NXS]], sq, b_sq, t + 2)
                        norm_rstd(t + 2)
                    norm_fin(xts[t % NXS], [b_xt[t % NXS]], g1col, hT[:, :, t * 128:(t + 1) * 128], b_hT[t // 4],
                             xns[t % 2], b_xn[t % 2], t, t % 2)
                S.barrier()
                if STOP_PHASE == 1:
                    raise _Stop()

                A1 = 64 * KB
                q_sb = view(A1, [128, 2, 2, U], BF16)
                k_sb = view(A1 + 16 * KB, [128, 2, EXT], BF16)
                v_sb = view(A1 + 32 * KB, [128, 32, 256], BF16)
                wqkv = [view(A1 + 48 * KB, [128, 8, 3, 256], BF16) for i in range(2)]
                b_q, b_k, b_v = Buf("q"), Buf("k"), Buf("v")
                _bw = [Buf("wqkv_%d" % j) for j in range(3)]
                b_wqkv = [_bw, _bw]
                S.op("dve", [lambda e: e.memset(q_sb[64:128, :, 0, :], 0.0), lambda e: e.memset(q_sb[0:64, :, 1, :], 0.0)], writes=[b_q])
                acc_od = view(128 * KB, [128, 4, U], F32)
                b_acc_od = Buf("acc_od")
                A3 = 160 * KB
                NSL = 4
                pTs = [view(A3 + i * KB, [128, 512], BF16) for i in range(NSL)]
                Ssb = [view(A3 + 4 * KB + i * 2 * KB, [128, 512], F32) for i in range(NSL)]
                ab_sb = view(A3 + 12 * KB, [128, 6, 512], F32)
                b_pT = [Buf("pT%d" % i) for i in range(NSL)]
                b_Ssb = [Buf("Ssb%d" % i) for i in range(NSL)]
                b_ab = Buf("abias")
                S.dma("sp", lambda e: e.dma_start(out=ab_sb, in_=abias_d.rearrange("p (a b) -> p a b", a=6)), writes=[b_ab], sembuf=b_ab)

                def load_qkv(g):
                    ws = wqkv[g % 2]
                    for j in range(3):
                        S.dma("pool", lambda e, j=j, ws=ws, g=g: e.dma_start(out=ws[:, :, j, :], in_=wcols(768 * j + 256 * g, 256)),
                              writes=[b_wqkv[g % 2][j]], sembuf=b_wqkv[g % 2][j])

                load_qkv(0)
                pbi = 0
                evac_i = 0
                for g in range(3):
                    dil = DIL[g]
                    ws = wqkv[g % 2]
                    bw = b_wqkv[g % 2]
                    kblocks = list(range(8)) if g == 2 else list(range(1, 7))
                    kbase = kblocks[0] * 512

                    def evac(dst, src, reads, writes):
                        nonlocal evac_i
                        evac_i += 1
                        if evac_i % 2 == 0:
                            S.op("act", lambda e: e.copy(out=dst, in_=src), reads=reads, writes=writes)
                        else:
                            S.op("dve", lambda e: e.tensor_copy(out=dst, in_=src), reads=reads, writes=writes)

                    for c in range(2):
                        for tb in range(4):
                            bk = pbi % 4
                            pbi += 1
                            t0 = HALO + tb * 512
                            S.op("pe", [lambda e, dc=dc, bk=bk, c=c, t0=t0: e.matmul(banks[bk][:, :], lhsT=ws[:, dc, 0, c * 128:(c + 1) * 128],
                                                                                  rhs=hT[:, dc, t0:t0 + 512], start=(dc == 0), stop=(dc == 7))
                                        for dc in range(8)], reads=[bw[0], b_hT[t0 // 512]], writes=[pb[bk]])
                            S.op("act", lambda e: e.copy(out=q_sb[0:64, c, 0, tb * 512:(tb + 1) * 512], in_=banks[bk][0:64, :]), reads=[pb[bk]], writes=[b_q])
                            S.op("dve", lambda e: e.tensor_copy(out=q_sb[64:128, c, 1, tb * 512:(tb + 1) * 512], in_=banks[bk][64:128, :]), reads=[pb[bk]], writes=[b_q])
                    if SUBSTOP == 'A':
                        raise _Stop()
                    for c in range(2):
                        for kbk in kblocks:
                            bk = pbi % 4
                            pbi += 1
                            t0 = kbk * 512
                            S.op("pe", [lambda e, dc=dc, bk=bk, c=c, t0=t0: e.matmul(banks[bk][:, :], lhsT=ws[:, dc, 1, c * 128:(c + 1) * 128],
                                                                                  rhs=hT[:, dc, t0:t0 + 512], start=(dc == 0), stop=(dc == 7))
                                        for dc in range(8)], reads=[bw[1], b_hT[kbk]], writes=[pb[bk]])
                            evac(k_sb[:, c, t0 - kbase:t0 - kbase + 512], banks[bk][:, :], [pb[bk]], [b_k])
                    if SUBSTOP == 'B':
                        raise _Stop()
                    nkt = NVT[g] // dil
                    vlist = [(r, kt) for r in range(dil) for kt in range(nkt)]
                    for i0 in range(0, len(vlist), 2):
                        bk = pbi % 4
                        pbi += 1
                        pair = vlist[i0:i0 + 2]
                        fns = []
                        for pi, (r, kt) in enumerate(pair):
                            s0 = HALO + r + dil * (128 * kt - 64)
                            for dc in range(8):
                                fns.append(lambda e, dc=dc, bk=bk, pi=pi, s0=s0: e.matmul(
                                    banks[bk][:, pi * 256:(pi + 1) * 256], lhsT=hT[:, dc, sl_(s0, dil)],
                                    rhs=ws[:, dc, 2, :], start=(dc == 0), stop=(dc == 7)))
                        S.op("pe", fns, reads=[bw[2]] + b_hT, writes=[pb[bk]])
                        vl0 = vt_local(g, pair[0][0], pair[0][1])
                        n = len(pair)
                        evac(v_sb[:, vl0:vl0 + n, :], banks[bk][:, 0:256 * n].rearrange("p (a b) -> p a b", a=n), [pb[bk]], [b_v])

                    if SUBSTOP == 'C':
                        raise _Stop()
                    if g + 1 < 3:
                        load_qkv(g + 1)
                    nq = U // dil // 128
                    steps = [(r, j, kk) for r in range(dil) for j in range(nq) for kk in range(2)]

                    def emit_S(i):
                        r, j, kk = steps[i]
                        kt = j + kk
                        bk = 2 + (i % NSL)
                        ks = HALO + r + dil * (128 * kt - 64) - kbase
                        qs = r + dil * 128 * j
                        fns = []
                        for h in range(4):
                            fns.append(lambda e, h=h, bk=bk, ks=ks, qs=qs: e.matmul(
                                banks[bk][:, h * 128:(h + 1) * 128], lhsT=k_sb[:, h // 2, sl_(ks, dil)],
                                rhs=q_sb[:, h // 2, h % 2, sl_(qs, dil)], start=True, stop=True))
                        S.op("pe", fns, reads=[b_k, b_q], writes=[pb[bk]])
                        sl = i % NSL
                        vt = VT_OFF[g] + vt_local(g, r, kt)
                        kcol = kbias[:, u * NVT_ALL + vt:u * NVT_ALL + vt + 1]
                        S.op("dve", lambda e: e.tensor_tensor(out=Ssb[sl], in0=banks[bk][:, :], in1=ab_sb[:, g * 2 + kk, :], op=ALU.add),
                             reads=[pb[bk], b_ab], writes=[b_Ssb[sl]])
                        S.op("act", lambda e: e.activation(out=pTs[sl], in_=Ssb[sl], func=AF.Exp, bias=kcol, scale=0.125),
                             reads=[b_Ssb[sl], b_const], writes=[b_pT[sl]])

                    def emit_rest(i):
                        r, j, kk = steps[i]
                        kt = j + kk
                        sl = i % NSL
                        vl = vt_local(g, r, kt)
                        nb = 6 + ((i // 2) % 2)
                        fns = []
                        for h in range(4):
                            fns.append(lambda e, h=h, nb=nb: e.matmul(
                                banks[nb][0:64, h * 128:(h + 1) * 128], lhsT=v_sb[:, vl, h * 64:(h + 1) * 64],
                                rhs=pTs[sl][:, h * 128:(h + 1) * 128], start=(kk == 0 and h == 0), stop=(kk == 1),
                                skip_group_check=True, tile_position=(0, 0)))
                        fns.append(lambda e, nb=nb: e.matmul(banks[nb][64:128, :], lhsT=ones_bf[:, :], rhs=pTs[sl][:, :],
                                                             start=(kk == 0), stop=(kk == 1), skip_group_check=True, tile_position=(0, 64)))
                        S.op("pe", fns, reads=[b_pT[sl], b_v, b_const], writes=[pb[nb]])
                        if kk == 1:
                            qs = r + dil * 128 * j
                            dst = acc_od[:, :, sl_(qs, dil)]
                            src = banks[nb][:, :].rearrange("p (a b) -> p a b", a=4)
                            if g == 0:
                                S.op("dve", lambda e: e.tensor_copy(out=dst, in_=src), reads=[pb[nb]], writes=[b_acc_od])
                            else:
                                S.op("dve", lambda e: e.tensor_tensor(out=dst, in0=dst, in1=src, op=ALU.add), reads=[pb[nb]], writes=[b_acc_od])

                    emit_S(0)
                    emit_S(1)
                    emit_S(2)
                    if SUBSTOP == 'D':
                        raise _Stop()
                    for i in range(len(steps)):
                        if i + 3 < len(steps):
                            emit_S(i + 3)
                        emit_rest(i)
                        if SUBSTOP == 'E' and i == 1:
                            raise _Stop()
                    if SUBSTOP == 'F':
                        raise _Stop()
                S.barrier()
                if STOP_PHASE == 2:
                    raise _Stop()

                attn_T = view(A3, [128, 4, U], BF16)
                ybT = view(A3 + 16 * KB, [128, 6, U], BF16)
                b_attn, b_yb = Buf("attn_T"), Buf("ybT")
                S.op("dve", lambda e: e.memset(attn_T[64:128, :, :], 0.0), writes=[b_attn])
                for tb in range(4):
                    blk = slice(tb * 512, (tb + 1) * 512)
                    S.op("act", lambda e, blk=blk: e.activation(out=acc_od[64:128, :, blk], in_=acc_od[64:128, :, blk], func=AF.Ln), writes=[b_acc_od])
                    S.op("act", lambda e, blk=blk: e.activation(out=acc_od[64:128, :, blk], in_=acc_od[64:128, :, blk], func=AF.Exp, scale=-1.0), writes=[b_acc_od])
                    for h in range(4):
                        bk = (tb * 4 + h) % 4
                        S.op("pe", lambda e, h=h, blk=blk, bk=bk: e.matmul(banks[bk][:, :], lhsT=shiftI[:, :], rhs=acc_od[:, h, blk],
                                                                         start=True, stop=True),
                             reads=[b_acc_od, b_const], writes=[pb[bk]])
                        S.op("dve", lambda e, h=h, blk=blk, bk=bk: e.tensor_tensor(out=attn_T[0:64, h, blk], in0=acc_od[0:64, h, blk],
                                                                                 in1=banks[bk][0:64, :], op=ALU.mult),
                             reads=[pb[bk], b_acc_od], writes=[b_attn])

                wcv = [view(A1 + i * 6 * KB, [128, 8, 3, 128], BF16) for i in range(2)]
                b_wcv = [[Buf("wcv%d_%d" % (i, j)) for j in range(3)] for i in range(2)]
                cg_sb = [view(A1 + 12 * KB + i * 2064, [128, 516], F32) for i in range(2)]
                u_sb = [view(A1 + 20 * KB + i * 2064, [128, 516], F32) for i in range(2)]
                c1_sb = [view(A1 + 28 * KB + i * 2 * KB, [128, 512], F32) for i in range(2)]
                b_cg = [Buf("cg%d" % i) for i in range(2)]
                b_u = [Buf("u%d" % i) for i in range(2)]
                b_c1 = [Buf("c1_%d" % i) for i in range(2)]

                def load_wcv(c):
                    for j in range(3):
                        S.dma("pool", lambda e, j=j, c=c: e.dma_start(out=wcv[c % 2][:, :, j, :], in_=wcols(2304 + 768 * j + 128 * c, 128)),
                              writes=[b_wcv[c % 2][j]], sembuf=b_wcv[c % 2][j])

                load_wcv(0)
                it = 0
                for c in range(6):
                    if c + 1 < 6:
                        load_wcv(c + 1)
                    wv_ = wcv[c % 2]
                    bwv = b_wcv[c % 2]
                    for tb in range(4):
                        sl = it % 2
                        bA, bB, bC, bD = [4 * sl + i for i in range(4)]
                        it += 1
                        t0 = HALO + tb * 512
                        hb = [b_hT[t0 // 512 - 1], b_hT[t0 // 512], b_hT[t0 // 512 + 1]]

                        def mm(bk, j, rhs_of, ncols, c0=0):
                            return [lambda e, dc=dc: e.matmul(banks[bk][:, c0:c0 + ncols], lhsT=wv_[:, dc, j, :], rhs=rhs_of(dc),
                                                              start=(dc == 0), stop=(dc == 7)) for dc in range(8)]
                        S.op("pe", mm(bA, 1, lambda dc: hT[:, dc, t0:t0 + 512], 512), reads=[bwv[1]] + hb, writes=[pb[bA]])
                        S.op("pe", mm(bB, 2, lambda dc: hT[:, dc, t0:t0 + 512], 512), reads=[bwv[2]] + hb, writes=[pb[bB]])
                        S.op("pe", mm(bC, 0, lambda dc: hT[:, dc, t0:t0 + 512], 512), reads=[bwv[0]] + hb, writes=[pb[bC]])
                        S.op("pe", mm(bD, 1, lambda dc: hT[:, dc, t0 - 1:t0 + 513:513], 2, 0)
                             + mm(bD, 2, lambda dc: hT[:, dc, t0 - 1:t0 + 513:513], 2, 2), reads=[bwv[1], bwv[2]] + hb, writes=[pb[bD]])
                        cg, uu, c1 = cg_sb[sl], u_sb[sl], c1_sb[sl]
                        S.op("act", [lambda e: e.copy(out=cg[:, 0:512], in_=banks[bA][:, :]),
                                     lambda e: e.copy(out=cg[:, 512:514], in_=banks[bD][:, 0:2])],
                             reads=[pb[bA], pb[bD]], writes=[b_cg[sl]])
                        S.op("dve", [lambda e: e.tensor_tensor(out=uu[:, 1:513], in0=cg[:, 0:512], in1=banks[bB][:, :], op=ALU.mult),
                                     lambda e: e.tensor_tensor(out=uu[:, 0:514:513], in0=cg[:, 512:514], in1=banks[bD][:, 2:4], op=ALU.mult)],
                             reads=[b_cg[sl], pb[bB], pb[bD]], writes=[b_u[sl]])
                        S.op("dve", lambda e: e.tensor_scalar(out=c1, in0=uu[:, 0:512], scalar1=cwcol[:, c, 0:1], scalar2=None, op0=ALU.mult),
                             reads=[b_u[sl], b_const], writes=[b_c1[sl]])
                        S.op("dve", lambda e: e.scalar_tensor_tensor(out=c1, in0=uu[:, 1:513], scalar=cwcol[:, c, 1:2], in1=c1, op0=ALU.mult, op1=ALU.add),
                             reads=[b_u[sl]], writes=[b_c1[sl]])
                        S.op("dve", lambda e: e.scalar_tensor_tensor(out=c1, in0=uu[:, 2:514], scalar=cwcol[:, c, 2:3], in1=c1, op0=ALU.mult, op1=ALU.add),
                             reads=[b_u[sl]], writes=[b_c1[sl]])
                        S.op("dve", lambda e: e.tensor_tensor(out=ybT[:, c, tb * 512:(tb + 1) * 512], in0=c1, in1=banks[bC][:, :], op=ALU.mult),
                             reads=[b_c1[sl], pb[bC]], writes=[b_yb])
                S.barrier()
                if STOP_PHASE == 3:
                    raise _Stop()

                acc = view(64 * KB, [128, NT_OWN, D], F32)
                b_acc = [Buf("acc%d" % i) for i in range(NT_OWN)]
                h2T = view(128 * KB, [128, 8, U], BF16)
                b_h2T = [Buf("h2T%d" % i) for i in range(4)]
                logits = view(200 * KB, [128, NT_OWN, 36], F32)
                comb = view(200 * KB + 2304, [128, NT_OWN, NE], F32)
                b_log, b_comb = Buf("logits"), Buf("comb")

                def mcol(tok):
                    return tok if tok < 1024 else 3072 + (tok - 1024)
                b_mrg = [Buf("mrg%d" % i) for i in range(4)]
                A2 = 128 * KB
                wg_b = [view(A2 + i * 4 * KB, [128, 8, 2, 128], BF16) for i in range(2)]
                wao_b = [view(A2 + 8 * KB + i * KB, [128, 4, 128], BF16) for i in range(2)]
                wco_b = [view(A2 + 10 * KB + i * 1536, [128, 6, 128], BF16) for i in range(2)]
                gts = [view(A2 + 14 * KB + i * 2 * KB, [128, 512], F32) for i in range(4)]
                b_xpre = Buf("xpre")
                for ot in range(NT_OWN):
                    S.dma("sp", lambda e, ot=ot: e.dma_start(out=acc[:, ot, :], in_=x_d[u, HALO + ot * 128:HALO + (ot + 1) * 128, :]),
                          writes=[b_acc[ot]], sembuf=b_xpre)
                for ot in range(NT_OWN):
                    b_acc[ot].w = (b_xpre.sem, b_xpre.cnt)
                b_bun = [[Buf("bun%d_%d" % (i, j)) for j in range(4)] for i in range(2)]
                b_gt = [Buf("gt%d" % i) for i in range(4)]

                def load_bundle(dcol):
                    sl = dcol % 2
                    S.dma("pool", lambda e: e.dma_start(out=wg_b[sl][:, :, 0, :], in_=wcols(4608 + dcol * 128, 128)), writes=[b_bun[sl][0]], sembuf=b_bun[sl][0])
                    S.dma("pool", lambda e: e.dma_start(out=wg_b[sl][:, :, 1, :], in_=wcols(4608 + D + dcol * 128, 128)), writes=[b_bun[sl][1]], sembuf=b_bun[sl][1])
                    S.dma("pool", lambda e: e.dma_start(out=wao_b[sl][0:64, :, :], in_=wao_d[:, dcol * 128:(dcol + 1) * 128].rearrange("(h e) n -> e h n", e=64)),
                          writes=[b_bun[sl][2]], sembuf=b_bun[sl][2])
                    S.dma("pool", lambda e: e.dma_start(out=wco_b[sl][:, :, :], in_=wco_d[:, dcol * 128:(dcol + 1) * 128].rearrange("(c p) n -> p c n", p=128)),
                          writes=[b_bun[sl][3]], sembuf=b_bun[sl][3])

                for _i in range(2):
                    S.op("dve", lambda e, _i=_i: e.memset(wao_b[_i][64:128, :, :], 0.0), writes=[b_bun[_i][2]])
                load_bundle(0)
                it4 = 0
                for dcol in range(8):
                    sl = dcol % 2
                    if dcol + 1 < 8:
                        load_bundle(dcol + 1)
                    for tb in range(4):
                        blk = slice(tb * 512, (tb + 1) * 512)
                        oblk = slice(HALO + tb * 512, HALO + (tb + 1) * 512)
                        mc = mcol(tb * 512)
                        ps = 4 * (it4 % 2)
                        gs = 2 * (it4 % 2)
                        it4 += 1
                        bYA, bYB, bG0, bG1 = ps, ps + 1, ps + 2, ps + 3
                        S.op("pe", [lambda e, h=h: e.matmul(banks[bYA][:, :], lhsT=wao_b[sl][:, h, :], rhs=attn_T[:, h, blk],
                                                            start=(h == 0), stop=(h == 3)) for h in range(4)],
                             reads=[b_bun[sl][2], b_attn], writes=[pb[bYA]])
                        S.op("pe", [lambda e, c=c: e.matmul(banks[bYB][:, :], lhsT=wco_b[sl][:, c, :], rhs=ybT[:, c, blk],
                                                            start=(c == 0), stop=(c == 5)) for c in range(6)],
                             reads=[b_bun[sl][3], b_yb], writes=[pb[bYB]])
                        S.op("pe", [lambda e, dc=dc: e.matmul(banks[bG0][:, :], lhsT=wg_b[sl][:, dc, 0, :], rhs=hT[:, dc, oblk],
                                                              start=(dc == 0), stop=(dc == 7)) for dc in range(8)],
                             reads=[b_bun[sl][0], b_hT[2 + tb]], writes=[pb[bG0]])
                        S.op("pe", [lambda e, dc=dc: e.matmul(banks[bG1][:, :], lhsT=wg_b[sl][:, dc, 1, :], rhs=hT[:, dc, oblk],
                                                              start=(dc == 0), stop=(dc == 7)) for dc in range(8)],
                             reads=[b_bun[sl][1], b_hT[2 + tb]], writes=[pb[bG1]])
                        g0s, g1s = gts[gs], gts[gs + 1]
                        S.op("act", lambda e: e.activation(out=g0s, in_=banks[bG0][:, :], func=AF.Sigmoid, bias=bgate[:, dcol:dcol + 1]),
                             reads=[pb[bG0], b_const], writes=[b_gt[gs]])
                        S.op("act", lambda e: e.activation(out=g1s, in_=banks[bG1][:, :], func=AF.Sigmoid, bias=bgate[:, 8 + dcol:9 + dcol]),
                             reads=[pb[bG1], b_const], writes=[b_gt[gs + 1]])
                        S.op("dve", lambda e: e.tensor_tensor(out=g0s, in0=g0s, in1=banks[bYA][:, :], op=ALU.mult),
                             reads=[pb[bYA]], writes=[b_gt[gs]])
                        S.op("dve", lambda e: e.tensor_tensor(out=g1s, in0=g1s, in1=banks[bYB][:, :], op=ALU.mult),
                             reads=[pb[bYB]], writes=[b_gt[gs + 1]])
                        S.op("dve", lambda e: e.tensor_tensor(out=hT[:, dcol, mc:mc + 512], in0=g0s, in1=g1s, op=ALU.add),
                             reads=[b_gt[gs], b_gt[gs + 1]], writes=[b_mrg[tb]])
                S.barrier()
                wout_sb = view(A3, [128, 8, D], BF16)
                xns4 = [view(A3 + 32 * KB + i * 2 * KB, [128, D], BF16) for i in range(2)]
                sq4 = view(A3 + 36 * KB, [128, D], BF16)
                b_wout = Buf("wout")
                b_xn4 = [Buf("xn4_%d" % i) for i in range(2)]
                b_sq4 = Buf("sq4")
                S.dma("pool", lambda e: e.dma_start(out=wout_sb, in_=wout_d.rearrange("(c p) n -> p c n", p=128)), writes=[b_wout], sembuf=b_wout)

                def p4_wout(ot):
                    mc = mcol(ot * 128)
                    for half in range(2):
                        bk = (ot % 2) * 2 + half
                        hs = slice(half * 512, (half + 1) * 512)
                        S.op("pe", [lambda e, dcol=dcol: e.matmul(banks[bk][:, :], lhsT=hT[:, dcol, mc:mc + 128],
                                                                  rhs=wout_sb[:, dcol, hs], start=(dcol == 0), stop=(dcol == 7))
                                    for dcol in range(8)], reads=[b_mrg[ot // 4], b_wout], writes=[pb[bk]])
                        S.op("dve", lambda e: e.tensor_tensor(out=acc[:, ot, hs], in0=banks[bk][:, :], in1=acc[:, ot, hs], op=ALU.add),
                             reads=[pb[bk]], writes=[b_acc[ot]])

                def p4_rest(ot):
                    tb = ot // 4
                    norm_fin(acc[:, ot, :], [b_acc[ot]], g2col, h2T[:, :, ot * 128:(ot + 1) * 128], b_h2T[tb],
                             xns4[ot % 2], b_xn4[ot % 2], ot, 4 + ot % 2)

                def p4_router(ot):
                    tb = ot // 4
                    bk = 6 + (ot % 2)
                    S.op("pe", [lambda e, dc=dc: e.matmul(banks[bk][:, 0:36], lhsT=h2T[:, dc, ot * 128:(ot + 1) * 128], rhs=wr_bf[:, dc, :],
                                                          start=(dc == 0), stop=(dc == 7)) for dc in range(8)],
                         reads=[b_h2T[tb], b_constp], writes=[pb[bk]])
                    S.op("dve", lambda e: e.tensor_tensor(out=logits[:, ot, :], in0=banks[bk][:, 0:36], in1=br_bc[:, :], op=ALU.add),
                         reads=[pb[bk], b_const], writes=[b_log])

                for ot in range(3):
                    p4_wout(ot)
                for ot in range(2):
                    norm_sq(acc[:, ot, :], [b_acc[ot]], sq4, b_sq4, ot)
                    norm_rstd(ot)
                for ot in range(NT_OWN):
                    if ot + 3 < NT_OWN:
                        p4_wout(ot + 3)
                    if ot + 2 < NT_OWN:
                        norm_sq(acc[:, ot + 2, :], [b_acc[ot + 2]], sq4, b_sq4, ot + 2)
                        norm_rstd(ot + 2)
                    p4_rest(ot)
                    if ot >= 1:
                        p4_router(ot - 1)
                p4_router(NT_OWN - 1)
                S.barrier()
                if STOP_PHASE == 4:
                    raise _Stop()

                T = NT_OWN
                R0 = 0
                def rv(off, shape):
                    return view(R0 + off, shape, F32)
                cmax = rv(0, [128, T])
                oh = rv(256, [128, T, 4])
                ctmp = rv(768, [128, T, 4])
                csum = rv(1280, [128, T])
                pg = rv(1536, [128, T])
                tmp4 = rv(2048, [128, T, 4, 8])
                fs = rv(4096, [128, T, 8])
                srt = rv(4608, [128, T, 8])
                m1 = rv(5120, [128, T, 8])
                m2 = rv(5632, [128, T, 8])
                dl = rv(6144, [128, T])
                sg = rv(6400, [128, T])
                w1v = rv(6656, [128, T])
                w2v = rv(6912, [128, T])
                wdv = rv(7168, [128, T])
                b_r = Buf("router")
                coarse = logits[:, :, 0:4]
                fine = logits[:, :, 4:36].rearrange("p t (g e) -> p t g e", g=4)
                RW = dict(reads=[b_log], writes=[b_r])
                S.op("dve", lambda e: e.tensor_reduce(out=cmax, in_=coarse, axis=AX.X, op=ALU.max), **RW)
                S.op("dve", lambda e: e.tensor_tensor(out=oh, in0=coarse, in1=cmax.unsqueeze(2).to_broadcast([128, T, 4]), op=ALU.is_equal), **RW)
                S.op("dve", lambda e: e.tensor_tensor(out=ctmp, in0=coarse, in1=cmax.unsqueeze(2).to_broadcast([128, T, 4]), op=ALU.subtract), **RW)
                S.op("act", lambda e: e.activation(out=ctmp, in_=ctmp, func=AF.Exp), **RW)
                S.op("dve", lambda e: e.tensor_reduce(out=csum, in_=ctmp, axis=AX.X, op=ALU.add), **RW)
                S.op("dve", lambda e: e.reciprocal(out=pg, in_=csum), **RW)
                S.op("dve", lambda e: e.tensor_tensor(out=tmp4, in0=fine, in1=oh.unsqueeze(3).to_broadcast([128, T, 4, 8]), op=ALU.mult), **RW)
                S.op("dve", lambda e: e.tensor_reduce(out=fs, in_=tmp4.rearrange("p t g e -> p t e g"), axis=AX.X, op=ALU.add), **RW)
                for t in range(T):
                    S.op("dve", lambda e, t=t: e.max(out=srt[:, t, :], in_=fs[:, t, :]), **RW)
                S.op("dve", lambda e: e.tensor_tensor(out=m1, in0=fs, in1=srt[:, :, 0:1].to_broadcast([128, T, 8]), op=ALU.is_equal), **RW)
                S.op("dve", lambda e: e.tensor_tensor(out=m2, in0=fs, in1=srt[:, :, 1:2].to_broadcast([128, T, 8]), op=ALU.is_ge), **RW)
                S.op("dve", lambda e: e.tensor_tensor(out=dl, in0=srt[:, :, 0], in1=srt[:, :, 1], op=ALU.subtract), **RW)
                S.op("act", lambda e: e.activation(out=sg, in_=dl, func=AF.Sigmoid), **RW)
                S.op("dve", lambda e: e.tensor_tensor(out=w1v, in0=pg, in1=sg, op=ALU.mult), **RW)
                S.op("dve", lambda e: e.tensor_tensor(out=w2v, in0=pg, in1=w1v, op=ALU.subtract), **RW)
                S.op("dve", lambda e: e.tensor_tensor(out=wdv, in0=w1v, in1=w2v, op=ALU.subtract), **RW)
                S.op("dve", lambda e: e.tensor_tensor(out=m1, in0=m1, in1=wdv.unsqueeze(2).to_broadcast([128, T, 8]), op=ALU.mult), **RW)
                S.op("dve", lambda e: e.tensor_tensor(out=m2, in0=m2, in1=w2v.unsqueeze(2).to_broadcast([128, T, 8]), op=ALU.mult), **RW)
                S.op("dve", lambda e: e.tensor_tensor(out=m1, in0=m1, in1=m2, op=ALU.add), **RW)
                S.op("dve", lambda e: e.tensor_tensor(out=comb.rearrange("p t (g e) -> p t g e", g=4),
                                                      in0=oh.unsqueeze(3).to_broadcast([128, T, 4, 8]),
                                                      in1=m1.unsqueeze(2).to_broadcast([128, T, 4, 8]), op=ALU.mult),
                     reads=[b_r], writes=[b_comb])
                S.barrier()
                if STOP_PHASE == 5:
                    raise _Stop()

                w1s = [view(i * 24 * KB, [128, 8, FF], BF16) for i in range(2)]
                w3s = [view(i * 24 * KB + 8 * KB, [128, 8, FF], BF16) for i in range(2)]
                w2s = [view(i * 24 * KB + 16 * KB, [128, 4, D], BF16) for i in range(2)]
                b_w = [[Buf("mw%d_%d" % (i, j)) for j in range(3)] for i in range(2)]
                hid = [view(48 * KB + i * 4 * KB, [128, 4, 512], BF16) for i in range(2)]
                b_hid = [Buf("hid%d" % i) for i in range(2)]
                sas = [view(56 * KB + i * 2 * KB, [128, 512], F32) for i in range(2)]
                b_sa = [Buf("sa%d" % i) for i in range(2)]

                def load_expert(ex):
                    sl = ex % 2
                    S.dma("pool", lambda e: e.dma_start(out=w1s[sl], in_=w1_d[ex].rearrange("(c p) n -> p c n", p=128)), writes=[b_w[sl][0]], sembuf=b_w[sl][0])
                    S.dma("pool", lambda e: e.dma_start(out=w3s[sl], in_=w3_d[ex].rearrange("(c p) n -> p c n", p=128)), writes=[b_w[sl][1]], sembuf=b_w[sl][1])
                    S.dma("pool", lambda e: e.dma_start(out=w2s[sl], in_=w2_d[ex].rearrange("(c p) n -> p c n", p=128)), writes=[b_w[sl][2]], sembuf=b_w[sl][2])

                msteps = [(ex, tb) for ex in range(NE) for tb in range(4)]
                sa_i = 0

                def emit_up(i):
                    nonlocal sa_i
                    ex, tb = msteps[i]
                    sl = ex % 2
                    hs = i % 2
                    blk = slice(tb * 512, (tb + 1) * 512)
                    for fc in range(4):
                        bA = 2 * (fc % 2)
                        bB = bA + 1
                        S.op("pe", [lambda e, dc=dc: e.matmul(banks[bA][:, :], lhsT=w1s[sl][:, dc, fc * 128:(fc + 1) * 128], rhs=h2T[:, dc, blk],
                                                              start=(dc == 0), stop=(dc == 7)) for dc in range(8)],
                             reads=[b_w[sl][0], b_h2T[tb]], writes=[pb[bA]])
                        S.op("pe", [lambda e, dc=dc: e.matmul(banks[bB][:, :], lhsT=w3s[sl][:, dc, fc * 128:(fc + 1) * 128], rhs=h2T[:, dc, blk],
                                                              start=(dc == 0), stop=(dc == 7)) for dc in range(8)],
                             reads=[b_w[sl][1], b_h2T[tb]], writes=[pb[bB]])
                        ss = sa_i % 2
                        sa_i += 1
                        S.op("act", lambda e: e.activation(out=sas[ss], in_=banks[bA][:, :], func=AF.Silu), reads=[pb[bA]], writes=[b_sa[ss]])
                        S.op("dve", lambda e: e.tensor_tensor(out=hid[hs][:, fc, :], in0=sas[ss], in1=banks[bB][:, :], op=ALU.mult),
                             reads=[b_sa[ss], pb[bB]], writes=[b_hid[hs]])

                def emit_down(i):
                    ex, tb = msteps[i]
                    sl = ex % 2
                    hs = i % 2
                    for tt in range(4):
                        ot = tb * 4 + tt
                        for half in range(2):
                            bk = 4 + (tt * 2 + half) % 4
                            cs = slice(half * 512, (half + 1) * 512)
                            S.op("pe", [lambda e, fc=fc: e.matmul(banks[bk][:, :], lhsT=hid[hs][:, fc, tt * 128:(tt + 1) * 128], rhs=w2s[sl][:, fc, cs],
                                                                  start=(fc == 0), stop=(fc == 3)) for fc in range(4)],
                                 reads=[b_hid[hs], b_w[sl][2]], writes=[pb[bk]])
                            S.op("dve", lambda e: e.scalar_tensor_tensor(out=acc[:, ot, cs], in0=banks[bk][:, :], scalar=comb[:, ot, ex:ex + 1],
                                                                         in1=acc[:, ot, cs], op0=ALU.mult, op1=ALU.add),
                                 reads=[pb[bk], b_comb], writes=[b_acc[ot]])

                load_expert(0)
                load_expert(1)
                emit_up(0)
                for i in range(len(msteps)):
                    if i + 1 < len(msteps):
                        emit_up(i + 1)
                    emit_down(i)
                    ex, tb = msteps[i]
                    if tb == 3 and ex + 2 < NE:
                        load_expert(ex + 2)
                S.barrier()
                if STOP_PHASE == 6:
                    raise _Stop()

                gF = view(0, [128, D], F32)
                b_gF = Buf("gF")
                ost = [view(4 * KB + i * 4 * KB, [128, D], F32) for i in range(4)]
                b_ost = [Buf("ost%d" % i) for i in range(4)]
                sq7 = view(20 * KB, [128, D], BF16)
                b_sq7 = Buf("sq7")
                S.dma("sp", lambda e: e.dma_start(out=gF, in_=gf_d.partition_broadcast(128)), writes=[b_gF], sembuf=b_gF)
                for ot in range(2):
                    norm_sq(acc[:, ot, :], [b_acc[ot]], sq7, b_sq7, ot)
                    norm_rstd(ot)
                for ot in range(NT_OWN):
                    sl = ot % 4
                    if ot + 2 < NT_OWN:
                        norm_sq(acc[:, ot + 2, :], [b_acc[ot + 2]], sq7, b_sq7, ot + 2)
                        norm_rstd(ot + 2)
                    st = b_stat[ot]
                    cc = stat_c[:, ot:ot + 1]
                    S.op("dve", lambda e: e.scalar_tensor_tensor(out=ost[sl], in0=acc[:, ot, :], scalar=cc, in1=gF, op0=ALU.mult, op1=ALU.mult),
                         reads=[b_acc[ot], st, b_gF], writes=[b_ost[sl]])
                    S.dma("sp", lambda e, ot=ot, sl=sl: e.dma_start(out=out_d[u * U + ot * 128:u * U + (ot + 1) * 128, :], in_=ost[sl]),
                          reads=[b_ost[sl]], sembuf=b_ost[sl])
                S.barrier()
                if STOP_PHASE == 7:
                    raise _Stop()
            except _Stop:
                pass
        for b in S.dma_bufs:
            S._wait("sp", (b.sem, b.cnt))
        for b in b_ost:
            for ev in list(b.r.values()):
                S._wait("sp", ev)

        block = es.enter_context(nc.Block())

        @block.tensor
        def _(e):
            S.replay("pe", e)

        @block.scalar
        def _(e):
            S.replay("act", e)

        @block.vector
        def _(e):
            S.replay("dve", e)

        @block.gpsimd
        def _(e):
            S.replay("pool", e)

        @block.sync
        def _(e):
            S.replay("sp", e)
    return nc


def alibi_tables():
    slopes = np.array([2.0 ** (-8.0 * (i + 1) / 12) for i in range(12)], dtype=np.float64).reshape(3, 4)
    kk_ = np.arange(128)[:, None]
    qq_ = np.arange(128)[None, :]
    tab = np.zeros((128, 3, 2, 4, 128), dtype=np.float32)
    for g in range(3):
        for kk in range(2):
            delta = kk_ - qq_ - 64 + 128 * kk
            valid = np.abs(delta) <= 64
            for h in range(4):
                b = -slopes[g, h] * np.abs(delta) * DIL[g] * 8.0
                tab[:, g, kk, h, :] = np.where(valid, b, MASKV * 8.0).astype(np.float32)
    return tab.reshape(128, -1)


def key_bias(unit_start, seq_len):
    kb = np.zeros((128, NVT_ALL), dtype=np.float32)
    p = np.arange(128)
    for g in range(3):
        dil = DIL[g]
        nkt = NVT[g] // dil
        for r in range(dil):
            for kt in range(nkt):
                pos = unit_start + r + dil * (128 * kt - 64 + p)
                ok = (pos >= 0) & (pos < seq_len)
                kb[:, VT_OFF[g] + vt_local(g, r, kt)] = np.where(ok, 0.0, MASKV)
    return kb


def make_core_inputs(x, weights, n_cores, NU, seq_len):
    B, Sx, _ = x.shape
    units_per_seq = Sx // U
    xp = np.zeros((B, Sx + 2 * HALO, D), dtype=np.float32)
    xp[:, HALO:HALO + Sx] = x
    shared = dict(weights)
    shared["norm_mix_g"] = np.ascontiguousarray(weights["norm_mix_g"].reshape(8, 128).T)
    shared["norm_ffn_g"] = np.ascontiguousarray(weights["norm_ffn_g"].reshape(8, 128).T)
    shared["b_gate"] = np.ascontiguousarray(weights["b_gate"].reshape(16, 128).T)
    shared["conv_w"] = np.ascontiguousarray(weights["conv_w"].reshape(3, 6, 128).transpose(2, 1, 0).reshape(128, 18))
    shared["ident"] = np.eye(128, dtype=np.float32)
    shared["abias"] = alibi_tables()
    sh = np.zeros((128, 128), dtype=np.float32)
    sh[64 + (np.arange(128) % 64), np.arange(128)] = 1.0
    shared["shiftI"] = sh
    maps = []
    for c in range(n_cores):
        xs = np.zeros((NU, EXT, D), dtype=np.float32)
        kb = np.zeros((128, NU * NVT_ALL), dtype=np.float32)
        for uu in range(NU):
            gu = c * NU + uu
            b, us = divmod(gu, units_per_seq)
            xs[uu] = xp[b, us * U:us * U + EXT]
            kb[:, uu * NVT_ALL:(uu + 1) * NVT_ALL] = key_bias(us * U, seq_len)
        m = dict(shared)
        m["x"] = xs
        m["kbias"] = kb
        maps.append(m)
    return maps


_NC_CACHE = {}


def run(x, weights, n_cores, NU):
    if NU not in _NC_CACHE:
        _NC_CACHE[NU] = build_nc(NU)
    nc = _NC_CACHE[NU]
    B, Sx, _ = x.shape
    maps = make_core_inputs(x, weights, n_cores, NU, Sx)
    res = run_bass_kernel_spmd(nc, maps, core_ids=list(range(n_cores)))
    outs = [r["out"] for r in res.results]
    return np.concatenate(outs, axis=0).reshape(B, Sx, D)


def kernel(x, norm_mix_g, w_in, b_gate, conv_w, w_attn_out, w_conv_out, w_out, norm_ffn_g,
           w_route_group, b_route_group, w_route_expert, b_route_expert, w1, w3, w2, norm_final_g):
    f = lambda a: np.ascontiguousarray(np.asarray(a, dtype=np.float32))
    weights = {
        "norm_mix_g": f(norm_mix_g)[0], "w_in": f(w_in)[0], "b_gate": f(b_gate)[0], "conv_w": f(conv_w)[0],
        "w_attn_out": f(w_attn_out)[0], "w_conv_out": f(w_conv_out)[0], "w_out": f(w_out)[0],
        "norm_ffn_g": f(norm_ffn_g)[0], "w_route_group": f(w_route_group)[0], "b_route_group": f(b_route_group)[0],
        "w_route_expert": f(w_route_expert)[0], "b_route_expert": f(b_route_expert)[0],
        "w1": f(w1)[0], "w3": f(w3)[0], "w2": f(w2)[0], "norm_final_g": f(norm_final_g),
    }
    x = f(x)
    return run(x, weights, N_CORES, 2).astype(np.float32)
```
